# Optimizing a Trainium2 kernel written in Bass

```python
import jax, jax.numpy as jnp
from jax import lax
import numpy as np

D_MODEL = 1024
BATCH = 4
SEQ = 4096
DEPTH = 1

MEM_LEN = 256
RMS_EPS = 1e-6
F_GROUPS = 8
F_GROUP_DIM = 64
F_WIDTH = F_GROUPS * F_GROUP_DIM
MLA_HEADS = 8
QK_NOPE_DIM = 64
QK_ROPE_DIM = 32
V_HEAD_DIM = 64
Q_LORA_RANK = 384
KV_LORA_RANK = 256
ROPE_THETA = 10000.0
Q_BLOCK = 128
MLA_OUT = MLA_HEADS * V_HEAD_DIM
MIX_WIDTH = F_WIDTH + MLA_OUT
IN_WIDTH = F_WIDTH + Q_LORA_RANK + KV_LORA_RANK + QK_ROPE_DIM
IN_SPLITS = (F_WIDTH, F_WIDTH + Q_LORA_RANK, F_WIDTH + Q_LORA_RANK + KV_LORA_RANK)
MEM_HEADS = 4
MEM_HEAD_DIM = D_MODEL // MEM_HEADS
N_EXPERTS = 16
CAPACITY_FACTOR = 2
D_EXPERT = D_MODEL

kernel_name = "hybrid_fourier_mla_ec_moe_encoder"


def rms_norm(x, g):
    xf = x.astype(jnp.float32)
    y = xf * lax.rsqrt(jnp.mean(xf * xf, axis=-1, keepdims=True) + RMS_EPS)
    return (y * g.astype(jnp.float32)).astype(x.dtype)


def rope_tables(positions):
    freqs = 1.0 / (ROPE_THETA ** (jnp.arange(0, QK_ROPE_DIM, 2, dtype=jnp.float32) / QK_ROPE_DIM))
    ang = positions.astype(jnp.float32)[..., None] * freqs
    return jnp.cos(ang), jnp.sin(ang)


def apply_rope(x, cos, sin):
    xf = x.astype(jnp.float32)
    x1, x2 = jnp.split(xf, 2, axis=-1)
    out = jnp.concatenate([x1 * cos - x2 * sin, x1 * sin + x2 * cos], axis=-1)
    return out.astype(x.dtype)


def fourier_mix(u_f, w_fourier):
    B, S, _ = u_f.shape
    ug = u_f.reshape(B, S, F_GROUPS, F_GROUP_DIM).astype(jnp.float32)
    z = jnp.fft.fft2(ug, axes=(1, 3), norm="ortho").real.astype(u_f.dtype)
    y = jnp.einsum("bsgc,gce->bsge", z, w_fourier)
    return y.reshape(B, S, F_WIDTH)


def mla_attention(u_q, u_kv, u_kr, cos, sin, g_q_lat, w_q_up, g_kv_lat, w_kv_up):
    B, S, _ = u_q.shape
    q = (rms_norm(u_q, g_q_lat) @ w_q_up).reshape(B, S, MLA_HEADS, QK_NOPE_DIM + QK_ROPE_DIM)
    q_nope, q_rope = q[..., :QK_NOPE_DIM], q[..., QK_NOPE_DIM:]
    q_rope = apply_rope(q_rope, cos[:, :, None, :], sin[:, :, None, :])
    kv = (rms_norm(u_kv, g_kv_lat) @ w_kv_up).reshape(B, S, MLA_HEADS, QK_NOPE_DIM + V_HEAD_DIM)
    k_nope, v = kv[..., :QK_NOPE_DIM], kv[..., QK_NOPE_DIM:]
    k_rope = apply_rope(u_kr, cos, sin)
    scale = (QK_NOPE_DIM + QK_ROPE_DIM) ** -0.5
    nb = S // Q_BLOCK
    qn = q_nope.reshape(B, nb, Q_BLOCK, MLA_HEADS, QK_NOPE_DIM).swapaxes(0, 1)
    qr = q_rope.reshape(B, nb, Q_BLOCK, MLA_HEADS, QK_ROPE_DIM).swapaxes(0, 1)

    def block(args):
        qn_b, qr_b = args
        s = (jnp.einsum("bqhd,bkhd->bhqk", qn_b, k_nope)
             + jnp.einsum("bqhd,bkd->bhqk", qr_b, k_rope))
        p = jax.nn.softmax(s.astype(jnp.float32) * scale, axis=-1).astype(v.dtype)
        return jnp.einsum("bhqk,bkhd->bqhd", p, v)

    o = lax.map(block, (qn, qr))
    return o.swapaxes(0, 1).reshape(B, S, MLA_OUT)


def memory_cross_attention(hq, mn, w_mem_q, w_mem_kv, w_mem_o):
    B, S, D = hq.shape
    M = mn.shape[1]
    q = (hq @ w_mem_q).reshape(B, S, MEM_HEADS, MEM_HEAD_DIM)
    k, v = jnp.split(mn @ w_mem_kv, 2, axis=-1)
    k = k.reshape(B, M, MEM_HEADS, MEM_HEAD_DIM)
    v = v.reshape(B, M, MEM_HEADS, MEM_HEAD_DIM)
    s = jnp.einsum("bshd,bmhd->bhsm", q, k).astype(jnp.float32) * (MEM_HEAD_DIM ** -0.5)
    p = jax.nn.softmax(s, axis=-1).astype(v.dtype)
    o = jnp.einsum("bhsm,bmhd->bshd", p, v).reshape(B, S, D)
    return o @ w_mem_o


def expert_choice_moe(h, w_router, w_gate, w_up, w_down):
    B, S, D = h.shape
    cap = CAPACITY_FACTOR * S // N_EXPERTS
    aff = jax.nn.softmax((h @ w_router).astype(jnp.float32), axis=-1)
    gates, idx = lax.top_k(aff.swapaxes(1, 2), cap)
    xin = jax.vmap(lambda hb, ib: hb[ib])(h, idx)
    a = jnp.einsum("becd,edf->becf", xin, w_gate)
    b = jnp.einsum("becd,edf->becf", xin, w_up)
    y = jnp.einsum("becf,efd->becd", jax.nn.silu(a) * b, w_down)
    y = y * gates.astype(y.dtype)[..., None]
    out = jax.vmap(lambda ib, yb: jnp.zeros((S, D), yb.dtype).at[ib].add(yb))(
        idx.reshape(B, -1), y.reshape(B, -1, D))
    return out


def setup_inputs(seed: int = 0) -> dict:
    key = jax.random.key(seed)
    ks = jax.random.split(key, 24)
    f32 = jnp.float32

    def nrm(k, shape, fan_in):
        return jax.random.normal(k, shape, f32) * (fan_in ** -0.5)

    def gain(k, shape):
        return 1.0 + 0.02 * jax.random.normal(k, shape, f32)

    L = DEPTH
    x = jax.random.normal(ks[0], (BATCH, SEQ, D_MODEL), f32)
    mem = jax.random.normal(ks[1], (BATCH, MEM_LEN, D_MODEL), f32)
    offs = jax.random.randint(ks[2], (BATCH, 1), 0, 1024, dtype=jnp.int32)
    positions = offs + jnp.arange(SEQ, dtype=jnp.int32)[None, :]
    return {
        "x": x,
        "mem": mem,
        "positions": positions,
        "g_mix": gain(ks[3], (L, D_MODEL)),
        "w_in": nrm(ks[4], (L, D_MODEL, IN_WIDTH), D_MODEL),
        "g_q_lat": gain(ks[5], (L, Q_LORA_RANK)),
        "w_q_up": nrm(ks[6], (L, Q_LORA_RANK, MLA_HEADS * (QK_NOPE_DIM + QK_ROPE_DIM)), Q_LORA_RANK),
        "g_kv_lat": gain(ks[7], (L, KV_LORA_RANK)),
        "w_kv_up": nrm(ks[8], (L, KV_LORA_RANK, MLA_HEADS * (QK_NOPE_DIM + V_HEAD_DIM)), KV_LORA_RANK),
        "w_fourier": nrm(ks[9], (L, F_GROUPS, F_GROUP_DIM, F_GROUP_DIM), F_GROUP_DIM),
        "w_out": nrm(ks[10], (L, MIX_WIDTH, D_MODEL), MIX_WIDTH),
        "g_mem_q": gain(ks[11], (L, D_MODEL)),
        "g_mem_kv": gain(ks[12], (L, D_MODEL)),
        "w_mem_q": nrm(ks[13], (L, D_MODEL, D_MODEL), D_MODEL),
        "w_mem_kv": nrm(ks[14], (L, D_MODEL, 2 * D_MODEL), D_MODEL),
        "w_mem_o": nrm(ks[15], (L, D_MODEL, D_MODEL), D_MODEL),
        "g_ffn": gain(ks[16], (L, D_MODEL)),
        "w_router": nrm(ks[17], (L, D_MODEL, N_EXPERTS), D_MODEL),
        "w_exp_gate": nrm(ks[18], (L, N_EXPERTS, D_MODEL, D_EXPERT), D_MODEL),
        "w_exp_up": nrm(ks[19], (L, N_EXPERTS, D_MODEL, D_EXPERT), D_MODEL),
        "w_exp_down": nrm(ks[20], (L, N_EXPERTS, D_EXPERT, D_MODEL), D_EXPERT),
        "g_final": gain(ks[21], (D_MODEL,)),
    }


def reference(x, mem, positions, g_mix, w_in, g_q_lat, w_q_up, g_kv_lat, w_kv_up, w_fourier,
              w_out, g_mem_q, g_mem_kv, w_mem_q, w_mem_kv, w_mem_o, g_ffn, w_router,
              w_exp_gate, w_exp_up, w_exp_down, g_final):
    cos, sin = rope_tables(positions)
    for l in range(DEPTH):
        h = rms_norm(x, g_mix[l])
        u = h @ w_in[l]
        u_f, u_q, u_kv, u_kr = jnp.split(u, IN_SPLITS, axis=-1)
        y_f = fourier_mix(u_f, w_fourier[l])
        y_a = mla_attention(u_q, u_kv, u_kr, cos, sin, g_q_lat[l], w_q_up[l], g_kv_lat[l], w_kv_up[l])
        x = x + jnp.concatenate([y_f, y_a], axis=-1) @ w_out[l]
        x = x + memory_cross_attention(rms_norm(x, g_mem_q[l]), rms_norm(mem, g_mem_kv[l]),
                                       w_mem_q[l], w_mem_kv[l], w_mem_o[l])
        x = x + expert_choice_moe(rms_norm(x, g_ffn[l]), w_router[l],
                                  w_exp_gate[l], w_exp_up[l], w_exp_down[l])
    return rms_norm(x, g_final)
```

```python
import math, os
import numpy as np
import ml_dtypes
from contextlib import ExitStack
import concourse.bass as bass
import concourse.mybir as mybir
from concourse.bass_utils import run_bass_kernel_spmd

F32 = mybir.dt.float32
BF16 = mybir.dt.bfloat16
I32 = mybir.dt.int32
AF = mybir.ActivationFunctionType
ALU = mybir.AluOpType
AX = mybir.AxisListType

S = 4096
D = 1024
T = 32
EPS = 1e-6
NE = 16
CAP = 512
ROW = 1064
BIG = 100000.0


class KB:
    SEM_ROLL = 24000

    def __init__(self, nc, stack):
        self.nc = nc
        self.stack = stack
        self.engs = {"pe": nc.tensor, "act": nc.scalar, "dve": nc.vector,
                     "pool": nc.gpsimd, "sp": nc.sync}
        self.esem = {}
        self.nsem = 0
        for e in self.engs:
            self._new_esem(e)
        self.dsem = {}
        self.waited = {}
        self.lastw = {}
        self.readers = {}

    def _mk(self, name):
        self.nsem += 1
        return self.stack.enter_context(self.nc.semaphore(name))

    def _new_esem(self, e):
        gen = 0 if e not in self.esem else self.esem[e][2] + 1
        self.esem[e] = [self._mk(f"s_{e}_{gen}"), 0, gen]

    def _deps(self, reads, writes):
        deps = []
        for k in reads:
            t = self.lastw.get(k)
            if t is not None:
                deps.append(t)
        for k in writes:
            t = self.lastw.get(k)
            if t is not None:
                deps.append(t)
            deps.extend(self.readers.get(k, ()))
        return deps

    def _wait(self, e, deps, skip_self=False):
        best = {}
        for (h, v, pe) in deps:
            if skip_self and pe == e:
                continue
            if h.name not in best or best[h.name][1] < v:
                best[h.name] = (h, v)
        for nm, (h, v) in best.items():
            if self.waited.get((e, nm), 0) >= v:
                continue
            self.engs[e].wait_ge(h, v)
            self.waited[(e, nm)] = v

    def _record(self, tok, reads, writes):
        for k in writes:
            self.lastw[k] = tok
            self.readers[k] = []
        for k in reads:
            self.readers.setdefault(k, []).append(tok)

    def op(self, e, fn, reads=(), writes=(), signal=True, extra=()):
        writes = list(writes) + [k for k in reads if k.startswith("ps:") and k not in writes]
        deps = self._deps(reads, writes) + list(extra)
        self._wait(e, deps, skip_self=(e == "pe"))
        ins = fn()
        s = self.esem[e]
        if signal:
            s[1] += 1
            ins.then_inc(s[0], 1)
            tok = (s[0], s[1], e)
            if s[1] >= self.SEM_ROLL:
                self._new_esem(e)
        else:
            tok = (s[0], s[1] + 1, e)
        self._record(tok, reads, writes)
        return tok

    def _dsem(self, sem):
        if sem not in self.dsem:
            self.dsem[sem] = [self._mk(f"d_{sem}"), 0]
        s = self.dsem[sem]
        if s[1] >= self.SEM_ROLL:
            self.dsem[sem] = s = [self._mk(f"d_{sem}_{self.nsem}"), 0]
        return s

    def dma(self, q, out, in_, reads=(), writes=(), sem="d0", extra=(), **kw):
        deps = self._deps(reads, writes) + list(extra)
        self._wait(q, deps)
        s = self._dsem(sem)
        ins = self.engs[q].dma_start(out=out, in_=in_, **kw)
        s[1] += 16
        ins.then_inc(s[0], 16)
        tok = (s[0], s[1], "dma")
        self._record(tok, reads, writes)
        return tok

    def dma_ins(self, q, fn, reads=(), writes=(), sem="d0", extra=()):
        deps = self._deps(reads, writes) + list(extra)
        self._wait(q, deps)
        s = self._dsem(sem)
        ins = fn()
        s[1] += 16
        ins.then_inc(s[0], 16)
        tok = (s[0], s[1], "dma")
        self._record(tok, reads, writes)
        return tok

    def join(self, keys, sem):
        s = self.dsem[sem]
        tok = (s[0], s[1], "dma")
        for k in keys:
            self.lastw[k] = tok

    def all_tokens(self):
        toks = [(s[0], s[1], e) for e, s in self.esem.items() if s[1] > 0]
        toks += [(s[0], s[1], "dma") for s in self.dsem.values() if s[1] > 0]
        return toks

    def barrier(self, engines=("pe", "act", "dve", "pool", "sp")):
        toks = self.all_tokens()
        for e in engines:
            self._wait(e, toks)


def _bf(a):
    return np.ascontiguousarray(a).astype(ml_dtypes.bfloat16)


def host_consts():
    c = {}
    c["ident_bf"] = _bf(np.eye(128))
    c["ident_f"] = np.eye(128, dtype=np.float32)
    c["ones_bf"] = _bf(np.ones((128, 128)))
    c["ones_f"] = np.ones((128, 128), np.float32)
    c["ustrict"] = _bf(np.triu(np.ones((128, 128)), 1))
    i64 = np.arange(64)
    a = 2 * np.pi * np.outer(i64, i64) / 64.0
    f1 = np.concatenate([np.cos(a), -np.sin(a)], axis=1)
    c["f1"] = _bf(np.concatenate([f1, f1], axis=0))
    s2 = i64[:, None, None]
    k1 = i64[None, :, None]
    k2 = i64[None, None, :]
    ang = 2 * np.pi * s2 * (k1 + 64 * k2) / 4096.0
    wr, wi = np.cos(ang), -np.sin(ang)
    ta = np.concatenate([wr, wi], axis=2)
    tb = np.concatenate([-wi, wr], axis=2)
    c["tabA"] = _bf(np.concatenate([ta, ta], axis=0))
    c["tabB"] = _bf(np.concatenate([tb, tb], axis=0))
    lb = np.zeros((4, 128, 128), np.float64)
    for c2 in range(2):
        for ri in range(2):
            for j in range(2):
                for i in range(32):
                    ch = 2 * i + c2
                    m = np.arange(64)
                    coef = (np.cos if ri == 0 else np.sin)(2 * np.pi * ch * m / 64.0) / 512.0
                    for oc in range(2):
                        lb[c2 * 2 + ri, j * 64:(j + 1) * 64, oc * 64 + j * 32 + i] = coef
    c["lbig"] = _bf(lb.transpose(1, 0, 2))
    fr = 1.0 / (10000.0 ** (np.arange(0, 32, 2, dtype=np.float32) / 32.0))
    c["freq"] = np.ascontiguousarray(np.broadcast_to(fr.astype(np.float32)[None, :], (128, 16)))
    c["eoff"] = np.ascontiguousarray(np.broadcast_to((512.0 * np.arange(16, dtype=np.float32))[None, None, :], (128, 16, 16)))
    return c


def host_layout(inp, b, j):
    m = {}
    m["x"] = np.ascontiguousarray(inp["x"][b])
    m["xh"] = np.ascontiguousarray(inp["x"][b][2048 * j:2048 * (j + 1)])
    m["mem"] = np.ascontiguousarray(inp["mem"][b])
    m["posT"] = np.ascontiguousarray(inp["positions"][b].reshape(32, 128).T)
    w_in = inp["w_in"][0]
    kr = w_in[:, 1152:1184]
    kr_sw = np.concatenate([kr[:, 16:32], kr[:, 0:16]], axis=1)
    m["w_in"] = np.ascontiguousarray(np.concatenate(
        [w_in[:, 256 * j:256 * (j + 1)], w_in[:, 512:1184], kr_sw], axis=1))
    wq = inp["w_q_up"][0].reshape(384, 8, 96)
    hs = wq[:, 4 * j:4 * j + 4, :]
    rope = hs[:, :, 64:96]
    sw = np.concatenate([rope[:, :, 16:32], rope[:, :, 0:16]], axis=2).reshape(384, 128)
    m["w_q"] = np.ascontiguousarray(np.concatenate([hs.reshape(384, 384), sw], axis=1))
    wkv = inp["w_kv_up"][0].reshape(256, 8, 128)[:, 4 * j:4 * j + 4, :]
    m["w_kv"] = np.ascontiguousarray(np.concatenate(
        [wkv[:, :, 0:64].reshape(256, 256), wkv[:, :, 64:128].reshape(256, 256)], axis=1))
    wf = inp["w_fourier"][0]
    r = np.zeros((2, 128, 128), np.float32)
    for oc in range(2):
        for jj in range(2):
            r[oc, jj * 64:(jj + 1) * 64, jj * 64:(jj + 1) * 64] = wf[4 * j + 2 * oc + jj]
    m["wf_r"] = np.ascontiguousarray(r.transpose(1, 0, 2))
    m["w_out"] = np.ascontiguousarray(inp["w_out"][0])
    m["w_mem_q"] = np.ascontiguousarray(inp["w_mem_q"][0])
    m["w_mem_kv"] = np.ascontiguousarray(inp["w_mem_kv"][0])
    m["w_mem_o"] = np.ascontiguousarray(inp["w_mem_o"][0])
    m["w_router"] = np.ascontiguousarray(inp["w_router"][0])
    m["w_gate"] = np.ascontiguousarray(inp["w_exp_gate"][0][8 * j:8 * j + 8])
    m["w_up"] = np.ascontiguousarray(inp["w_exp_up"][0][8 * j:8 * j + 8])
    m["w_down"] = np.ascontiguousarray(inp["w_exp_down"][0][8 * j:8 * j + 8])
    rep = lambda v: np.ascontiguousarray(np.broadcast_to(v[None, :], (128, v.shape[0])))
    m["g_mix"] = rep(inp["g_mix"][0])
    m["g_mem_q"] = rep(inp["g_mem_q"][0])
    m["g_mem_kv"] = rep(inp["g_mem_kv"][0])
    m["g_ffn"] = rep(inp["g_ffn"][0])
    m["g_final"] = rep(inp["g_final"])
    m["g_q_lat"] = np.ascontiguousarray(inp["g_q_lat"][0].reshape(3, 128).T)
    m["g_kv_lat"] = np.ascontiguousarray(inp["g_kv_lat"][0].reshape(2, 128).T)
    p = np.arange(128)[:, None]
    i32 = lambda a: np.ascontiguousarray(a).astype(np.int32)
    cks = np.array([2 * j, 2 * j + 1, 4 + 2 * j, 4 + 2 * j + 1])[None, :]
    m["yidx"] = i32((cks * 128 + p) * 8)
    m["gidx"] = i32((np.arange(8)[None, :] * 128 + p) * 8 + 4 * j)
    m["hidx"] = i32(j * 128 + p)
    m["aidx"] = i32(np.concatenate([j * 128 + p, (1 - j) * 128 + p], axis=1))
    el = np.arange(8)[None, :, None]
    st_ = np.arange(4)[None, None, :]
    m["xidx"] = i32(((8 * j + el) * 512 + st_ * 128 + p[:, :, None]).reshape(128, 32))
    tl = np.arange(16)[None, :]
    gid = 2048 * j + tl * 128 + p
    oid = 2048 * (1 - j) + tl * 128 + p
    m["tokid"] = i32(gid)
    m["x2idx"] = i32(4096 * j + gid)
    m["zidx"] = i32(4096 * j + oid)
    m["f0idx"] = i32(gid)
    m["f1idx"] = i32(4096 + gid)
    m["jf"] = np.full((128, 1), float(j), np.float32)
    m["joff"] = np.full((128, 1), 4096.0 * j, np.float32)
    return m


IN_SPECS = [
    ("x", [S, D], F32), ("xh", [S // 2, D], F32), ("mem", [256, D], F32), ("posT", [128, 32], I32),
    ("w_in", [D, 960], F32), ("w_q", [384, 512], F32), ("w_kv", [256, 512], F32),
    ("wf_r", [128, 2, 128], F32), ("w_out", [D, D], F32), ("w_mem_q", [D, D], F32),
    ("w_mem_kv", [D, 2048], F32), ("w_mem_o", [D, D], F32), ("w_router", [D, NE], F32),
    ("w_gate", [8, D, D], F32), ("w_up", [8, D, D], F32), ("w_down", [8, D, D], F32),
    ("g_mix", [128, D], F32), ("g_mem_q", [128, D], F32), ("g_mem_kv", [128, D], F32),
    ("g_ffn", [128, D], F32), ("g_final", [128, D], F32), ("g_q_lat", [128, 3], F32),
    ("g_kv_lat", [128, 2], F32),
    ("ident_bf", [128, 128], BF16), ("ident_f", [128, 128], F32), ("ones_bf", [128, 128], BF16),
    ("ones_f", [128, 128], F32), ("ustrict", [128, 128], BF16), ("f1", [128, 128], BF16),
    ("tabA", [128, 64, 128], BF16), ("tabB", [128, 64, 128], BF16), ("lbig", [128, 4, 128], BF16),
    ("freq", [128, 16], F32), ("eoff", [128, 16, 16], F32),
    ("yidx", [128, 4], I32), ("gidx", [128, 8], I32), ("hidx", [128, 1], I32), ("aidx", [128, 2], I32),
    ("xidx", [128, 32], I32), ("tokid", [128, 16], I32), ("x2idx", [128, 16], I32), ("zidx", [128, 16], I32),
    ("f0idx", [128, 16], I32), ("f1idx", [128, 16], I32), ("jf", [128, 1], F32), ("joff", [128, 1], F32),
]
HT = 16


def build(upto=99, dbg=None):
    nc = bass.Bass("TRN2", target_bir_lowering=False)
    A = {}
    for name, shape, dt in IN_SPECS:
        A[name] = nc.dram_tensor(name, shape, dt, kind="ExternalInput").ap()
    out_d = nc.dram_tensor("out", [S // 2, D], F32, kind="ExternalOutput").ap()
    uT_d = nc.dram_tensor("uT_d", [6, 128, S], BF16).ap()
    yT_sh = nc.dram_tensor("yT_sh", [8 * 128 * 8, 512], BF16, addr_space="Shared").ap()
    affs_sh = nc.dram_tensor("affs_sh", [256, 256], F32, addr_space="Shared").ap()
    xin_sh = nc.dram_tensor("xin_sh", [NE * CAP, ROW], BF16, addr_space="Shared").ap()
    accp_sh = nc.dram_tensor("accp_sh", [2 * S, D], F32, addr_space="Shared").ap()
    bar_in = [nc.dram_tensor(f"bar_in{i}", [16, 16], F32).ap() for i in range(5)]
    bar_out = [nc.dram_tensor(f"bar_out{i}", [32, 16], F32).ap() for i in range(5)]
    dbg_out = {}

    with ExitStack() as st:
        kb = KB(nc, st)
        E = kb.engs

        def sb(stack, n, s, d):
            return stack.enter_context(nc.sbuf_tensor("sb_" + n, s, d))

        def ps(stack, n, s, d):
            return stack.enter_context(nc.psum_tensor("ps_" + n, s, d))

        def dump(name, src_ap, shape, dt, reads):
            o = nc.dram_tensor("dbg_" + name, shape, dt, kind="ExternalOutput").ap()
            dbg_out[name] = o
            kb.barrier()
            kb.dma("sp", o, src_ap, reads=reads, writes=["dbg_" + name], sem="dbg")

        def finish():
            kb.barrier()
            return nc, dbg_out

        def pair_barrier(i):
            kb.barrier()
            kb.dma("sp", bar_in[i], ones_f[0:16, 0:16], reads=["ones_f"], writes=[f"bar_in{i}"], sem="bar")
            kb.op("pool", lambda: nc.gpsimd.collective_compute(
                "AllGather", ALU.bypass, replica_groups=[[0, 1], [2, 3], [4, 5], [6, 7]],
                ins=[bar_in[i]], outs=[bar_out[i]]), reads=[f"bar_in{i}"], writes=[f"bar_out{i}"])
            kb.barrier()

        ident_bf = sb(st, "ident_bf", [128, 128], BF16)
        ident_f = sb(st, "ident_f", [128, 128], F32)
        ones_bf = sb(st, "ones_bf", [128, 128], BF16)
        ones_f = sb(st, "ones_f", [128, 128], F32)
        ustrict = sb(st, "ustrict", [128, 128], BF16)
        ssall = sb(st, "ssall", [128, 6, T], F32)
        rsall = sb(st, "rsall", [128, 6, T], F32)
        affl = sb(st, "affl", [128, HT, NE], F32)
        affs = sb(st, "affs", [128, T, NE], F32)
        posi = sb(st, "posi", [128, HT, NE], I32)
        jf = sb(st, "jf", [128, 1], F32)
        joff = sb(st, "joff", [128, 1], F32)
        yidx = sb(st, "yidx", [128, 4], I32)
        kb.dma("sp", jf[:], A["jf"], writes=["jf"], sem="c0")
        kb.dma("sp", joff[:], A["joff"], writes=["joff"], sem="c0")
        kb.dma("sp", yidx[:], A["yidx"], writes=["yidx"], sem="c0")
        kb.join(["ident_bf", "ident_f", "ones_bf", "ones_f", "ustrict", "jf", "joff", "yidx"], "c0")
        rq = sb(st, "rq", [128, T], F32)
        rkv = sb(st, "rkv", [128, T], F32)
        for nm, t in (("ident_bf", ident_bf), ("ident_f", ident_f), ("ones_bf", ones_bf),
                      ("ones_f", ones_f), ("ustrict", ustrict)):
            kb.dma("sp", t[:], A[nm], writes=[nm], sem="c0")
        kb.op("pool", lambda: nc.gpsimd.memset(ssall[:], 0.0), writes=["ssall"])
        if upto <= -3:
            return finish()

        big1 = sb(st, "big1", [128, 34816], BF16)
        sAD = ExitStack()
        cos2 = sb(sAD, "cos2", [128, T, 32], F32)
        sin2s = sb(sAD, "sin2s", [128, T, 32], F32)
        with ExitStack() as s0:
            posT = sb(s0, "posT", [128, T], I32)
            posf = sb(s0, "posf", [128, T], F32)
            freq = sb(s0, "freq", [128, 16], F32)
            ang = sb(s0, "ang", [128, T, 16], F32)
            kk = sb(s0, "kk", [128, T, 16], I32)
            kf = sb(s0, "kf", [128, T, 16], F32)
            rr = sb(s0, "rr", [128, T, 16], F32)
            mk = sb(s0, "mk", [128, T, 16], F32)
            sn = sb(s0, "sn", [128, T, 16], F32)
            kb.dma("sp", posT[:], A["posT"], writes=["posT"], sem="c0p")
            kb.dma("sp", freq[:], A["freq"], writes=["freq"], sem="c0f")
            kb.op("dve", lambda: nc.vector.tensor_copy(posf[:], posT[:]), reads=["posT"], writes=["posf"])
            kb.op("dve", lambda: nc.vector.tensor_tensor(
                ang[:], posf[:].unsqueeze(2).to_broadcast([128, T, 16]),
                freq[:].unsqueeze(1).to_broadcast([128, T, 16]), ALU.mult),
                reads=["posf", "freq"], writes=["ang"])
            if upto <= -2:
                dump("ang", ang[:], [128, T, 16], F32, ["ang"])
                return finish()

            def sin_of(shift, outs):
                TWO_PI = 2.0 * math.pi
                kb.op("dve", lambda: nc.vector.tensor_scalar(kf[:], ang[:], shift, 1.0 / TWO_PI, ALU.add, ALU.mult),
                      reads=["ang"], writes=["kf"])
                kb.op("dve", lambda: nc.vector.tensor_copy(kk[:], kf[:]), reads=["kf"], writes=["kk"])
                kb.op("dve", lambda: nc.vector.tensor_copy(kf[:], kk[:]), reads=["kk"], writes=["kf"])
                kb.op("dve", lambda: nc.vector.scalar_tensor_tensor(rr[:], kf[:], -TWO_PI, ang[:], ALU.mult, ALU.add),
                      reads=["kf", "ang"], writes=["rr"])
                if shift != 0.0:
                    kb.op("dve", lambda: nc.vector.tensor_scalar(rr[:], rr[:], shift, None, ALU.add),
                          reads=["rr"], writes=["rr"])
                for _ in range(2):
                    kb.op("dve", lambda: nc.vector.tensor_scalar(mk[:], rr[:], math.pi, -TWO_PI, ALU.is_gt, ALU.mult),
                          reads=["rr"], writes=["mk"])
                    kb.op("dve", lambda: nc.vector.tensor_tensor(rr[:], rr[:], mk[:], ALU.add),
                          reads=["rr", "mk"], writes=["rr"])
                    kb.op("dve", lambda: nc.vector.tensor_scalar(mk[:], rr[:], -math.pi, TWO_PI, ALU.is_lt, ALU.mult),
                          reads=["rr"], writes=["mk"])
                    kb.op("dve", lambda: nc.vector.tensor_tensor(rr[:], rr[:], mk[:], ALU.add),
                          reads=["rr", "mk"], writes=["rr"])
                kb.op("dve", lambda: nc.vector.tensor_scalar(rr[:], rr[:], math.pi, -math.pi, ALU.min, ALU.max),
                      reads=["rr"], writes=["rr"])
                if upto == -1:
                    return
                kb.op("act", lambda: nc.scalar.activation(sn[:], rr[:], AF.Sin), reads=["rr"], writes=["sn"])
                for dst, sign in outs:
                    kb.op("dve", lambda dst=dst, sign=sign: nc.vector.tensor_scalar(dst, sn[:], sign, None, ALU.mult),
                          reads=["sn"], writes=["rope_tab"])

            sin_of(0.0, [(sin2s[:, :, 0:16], -1.0), (sin2s[:, :, 16:32], 1.0)])
            if upto == -1:
                dump("rr", rr[:], [128, T, 16], F32, ["rr"])
                dump("ang", ang[:], [128, T, 16], F32, ["ang"])
                return finish()
            sin_of(0.5 * math.pi, [(cos2[:, :, 0:16], 1.0), (cos2[:, :, 16:32], 1.0)])
            kb.barrier()
        if upto <= 0:
            if dbg:
                dump("cos2", cos2[:], [128, T, 32], F32, ["rope_tab"])
                dump("sin2s", sin2s[:], [128, T, 32], F32, ["rope_tab"])
            return finish()

        hT = big1[:, 0:8 * S].rearrange("p (c s) -> p c s", c=8)
        with ExitStack() as sU:
            U = sb(sU, "U", [64, 256, 64], BF16)
            f1 = sb(sU, "f1", [128, 128], BF16)
            tabA = sb(sU, "tabA", [128, 64, 128], BF16)
            tabB = sb(sU, "tabB", [128, 64, 128], BF16)
            lbig = sb(sU, "lbig", [128, 4, 128], BF16)
            kb.dma("sp", f1[:], A["f1"], writes=["f1"], sem="c1")
            kb.dma("sp", tabA[:], A["tabA"], writes=["tabA"], sem="c1")
            kb.dma("sp", tabB[:], A["tabB"], writes=["tabB"], sem="c1")
            kb.dma("sp", lbig[:], A["lbig"], writes=["lbig"], sem="c1")
            kb.join(["f1", "tabA", "tabB", "lbig"], "c1")
            with ExitStack() as s1:
                Wb = sb(s1, "Wb", [128, 8, 960], BF16)
                gmix = sb(s1, "gmix", [128, D], F32)
                xt = [sb(s1, f"xt{i}", [128, D], F32) for i in range(2)]
                hb = [sb(s1, f"hb{i}", [128, D], BF16) for i in range(2)]
                sqj = sb(s1, "sqj", [128, D], BF16)
                sqt = [sb(s1, f"sqt{i}", [128, 512], BF16) for i in range(2)]
                ust = [sb(s1, f"ust{i}", [128, 512], BF16) for i in range(2)]
                tmpc = sb(s1, "tmpc", [128, 2 * T], F32)
                pT = [ps(s1, f"ps:pT{i}", [128, 8, 128], BF16) for i in range(2)]
                pB = [ps(s1, f"ps:pB{i}", [128, 512], F32) for i in range(2)]
                pss = ps(s1, "ps:pss", [128, 512], F32)
                for c in range(8):
                    kb.dma("pool", Wb[:, c, :], A["w_in"][c * 128:(c + 1) * 128, :], writes=[f"Wb_{c}"], sem="wb")
                kb.join(["Wb"], "wb")
                kb.dma("sp", gmix[:], A["g_mix"], writes=["gmix"], sem="c0g")
                kb.op("dve", lambda: nc.vector.memset(pss[:], 0.0), writes=["ps:pss"])
                def a_front(t):
                    i = t % 2
                    kb.dma("sp", xt[i][:], A["x"][t * 128:(t + 1) * 128, :], writes=[f"xt{i}"], sem=f"x{i}")
                    kb.op("act", lambda i=i, t=t: nc.scalar.activation(
                        sqj[:], xt[i][:], AF.Square, accum_out=ssall[:, 0, t:t + 1]),
                        reads=[f"xt{i}"], writes=["sqj", f"ssA{t}"])
                    kb.op("act", lambda t=t: nc.scalar.activation(
                        rsall[:, 0, t:t + 1], ssall[:, 0, t:t + 1], AF.Sqrt, bias=EPS, scale=1.0 / D),
                        reads=[f"ssA{t}"], writes=[f"rsA{t}"])
                    kb.op("dve", lambda t=t: nc.vector.reciprocal(rsall[:, 0, t:t + 1], rsall[:, 0, t:t + 1]),
                          reads=[f"rsA{t}"], writes=[f"rsA{t}"])
                    kb.op("dve", lambda i=i, t=t: nc.vector.scalar_tensor_tensor(
                        hb[i][:], xt[i][:], rsall[:, 0, t:t + 1], gmix[:], ALU.mult, ALU.mult),
                        reads=[f"xt{i}", f"rsA{t}", "gmix"], writes=[f"hb{i}"])

                def a_back(t):
                    i = t % 2
                    for c in range(8):
                        kb.op("pe", lambda i=i, c=c: nc.tensor.transpose(
                            pT[i][:, c, :], hb[i][:, c * 128:(c + 1) * 128], ident_bf[:]),
                            reads=[f"hb{i}", "ident_bf"], writes=[f"ps:pT{i}"], signal=(c == 7))
                    if t % 2 == 0:
                        kb.op("act", lambda i=i, t=t: nc.scalar.copy(hT[:, :, t * 128:(t + 1) * 128], pT[i][:]),
                              reads=[f"ps:pT{i}"], writes=[f"hT{t // 4}"])
                    else:
                        kb.op("dve", lambda i=i, t=t: nc.vector.tensor_copy(hT[:, :, t * 128:(t + 1) * 128], pT[i][:]),
                              reads=[f"ps:pT{i}"], writes=[f"hT{t // 4}"])

                a_front(0)
                for t in range(T):
                    if t + 1 < T:
                        a_front(t + 1)
                    a_back(t)
                if upto <= 1:
                    if dbg:
                        dump("hT", big1[:, 0:8 * S], [128, 8 * S], BF16, [f"hT{n}" for n in range(8)])
                    return finish()
                for s2 in range(64 if 'bf' not in os.environ.get('KSKIP', '') else 0):
                    i = s2 % 2
                    for c in range(8):
                        kb.op("pe", lambda i=i, c=c, s2=s2: nc.tensor.matmul(
                            pB[i][0:64, 0:256], hT[:, c, s2:S:64], Wb[:, c, 0:256],
                            start=(c == 0), stop=(c == 7)),
                            reads=[f"hT{n}" for n in range(8)] + ["Wb"], writes=[f"ps:pB{i}"],
                            signal=(c == 7))
                    if s2 % 2 == 0:
                        kb.op("act", lambda i=i, s2=s2: nc.scalar.copy(U[:, :, s2], pB[i][0:64, 0:256]),
                              reads=[f"ps:pB{i}"], writes=["U"])
                    else:
                        kb.op("dve", lambda i=i, s2=s2: nc.vector.tensor_copy(U[:, :, s2], pB[i][0:64, 0:256]),
                              reads=[f"ps:pB{i}"], writes=["U"])
                cnt = 0
                deferred = []
                for n in range(8):
                    for m in range(6):
                        M = 128 if m < 5 else 64
                        i = cnt % 2
                        cnt += 1
                        for c in range(8):
                            kb.op("pe", lambda i=i, m=m, M=M, c=c, n=n: nc.tensor.matmul(
                                pB[i][0:M, :], Wb[:, c, 256 + m * 128:256 + m * 128 + M],
                                hT[:, c, n * 512:(n + 1) * 512], start=(c == 0), stop=(c == 7)),
                                reads=[f"hT{n}", "Wb"], writes=[f"ps:pB{i}"], signal=(c == 7))
                        while deferred:
                            deferred.pop(0)()
                        tkc = kb.op("dve", lambda i=i, M=M: nc.vector.tensor_copy(ust[i][0:M, :], pB[i][0:M, :]),
                              reads=[f"ps:pB{i}"], writes=[f"ust{i}"])
                        if 'ud' not in os.environ.get('KSKIP', ''):
                            kb.dma("sp", uT_d[m, 0:M, n * 512:(n + 1) * 512], ust[i][0:M, :],
                                   reads=[f"ust{i}"], writes=["uT_d"], sem=f"us{i}")
                        if m < 5 and 'sq' not in os.environ.get('KSKIP', ''):
                            kb.op("act", lambda i=i: nc.scalar.activation(sqt[i][:], pB[i][:], AF.Square),
                                  reads=[f"ps:pB{i}"], writes=[f"sqt{i}"], extra=[tkc])
                            col0 = 0 if m < 3 else T

                            def ssq_mm(i=i, col0=col0, n=n):
                                for j in range(4):
                                    tcol = col0 + n * 4 + j
                                    kb.op("pe", lambda i=i, j=j, tcol=tcol: nc.tensor.matmul(
                                        pss[:, tcol:tcol + 1], sqt[i][:, j * 128:(j + 1) * 128], ones_bf[:, 0:1],
                                        start=False, stop=True, skip_group_check=True),
                                        reads=[f"sqt{i}", "ones_bf", "ps:pss"], writes=["ps:pss"], signal=(j == 3))
                            deferred.append(ssq_mm)
                while deferred:
                    deferred.pop(0)()
                kb.op("act", lambda: nc.scalar.activation(tmpc[:, 0:T], pss[:, 0:T], AF.Sqrt, bias=EPS, scale=1.0 / 384),
                      reads=["ps:pss"], writes=["tmpc"])
                kb.op("act", lambda: nc.scalar.activation(tmpc[:, T:2 * T], pss[:, T:2 * T], AF.Sqrt, bias=EPS, scale=1.0 / 256),
                      reads=["ps:pss"], writes=["tmpc"])
                kb.op("dve", lambda: nc.vector.reciprocal(rq[:], tmpc[:, 0:T]), reads=["tmpc"], writes=["rq"])
                kb.op("dve", lambda: nc.vector.reciprocal(rkv[:], tmpc[:, T:2 * T]), reads=["tmpc"], writes=["rkv"])
                kb.barrier()
            if upto <= 2:
                if dbg:
                    dump("U", U[:], [64, 256, 64], BF16, ["U"])
                    dump("uT", uT_d, [6, 128, S], BF16, ["uT_d"])
                    dump("rq", rq[:], [128, T], F32, ["rq"])
                    dump("rkv", rkv[:], [128, T], F32, ["rkv"])
                return finish()

            with ExitStack() as s2_:
                wfr = sb(s2_, "wfr", [128, 2, 128], BF16)
                Mb = sb(s2_, "Mb", [128, 8, 128], BF16)
                yst = [sb(s2_, f"yst{i}", [128, 512], BF16) for i in range(2)]
                pF = [ps(s2_, f"pF{i}", [128, 512], F32) for i in range(2)]
                pG = [ps(s2_, f"pG{i}", [128, 512], F32) for i in range(2)]
                pH = [ps(s2_, f"pH{i}", [128, 512], F32) for i in range(2)]
                Tt = big1[:, 0:16384].rearrange("p (cp k) -> p cp k", k=128)
                X = big1[:, 16384:32768].rearrange("p (c r k) -> p c r k", c=2, r=2)
                kb.dma("pool", wfr[:], A["wf_r"], writes=["wfr"], sem="c2")
                for og in range(2):
                    b = og % 2
                    for q4 in range(4):
                        kb.op("pe", lambda b=b, q4=q4, og=og: nc.tensor.matmul(
                            pF[b][:, q4 * 128:(q4 + 1) * 128], lbig[:, q4, :], wfr[:, og, :], start=True, stop=True),
                            reads=["lbig", "wfr"], writes=[f"ps:pF{b}"], signal=(q4 == 3))
                    ocl = og % 2
                    kb.op("dve", lambda b=b, og=og, ocl=ocl: nc.vector.tensor_copy(
                        Mb[64 * ocl:64 * ocl + 64, og * 4:(og + 1) * 4, :],
                        pF[b][64 * ocl:64 * ocl + 64, :].rearrange("p (q k) -> p q k", q=4)),
                        reads=[f"ps:pF{b}"], writes=["Mb"])
                ev = 0
                for fh in range(1):
                    for cp in range(128):
                        b = (cp // 4) % 2
                        c0 = fh * 256 + 2 * cp
                        kb.op("pe", lambda b=b, cp=cp, c0=c0: nc.tensor.matmul(
                            pF[b][:, (cp % 4) * 128:(cp % 4 + 1) * 128],
                            U[0:64, c0:c0 + 2, :].rearrange("p c s -> p (c s)"), f1[0:64, :], start=True, stop=True),
                            reads=["U", "f1"], writes=[f"ps:pF{b}"], signal=(cp % 4 == 3))
                        if cp % 4 == 3:
                            eng = "act" if ev % 2 == 0 else "dve"
                            ev += 1
                            dst = Tt[:, cp - 3:cp + 1, :]
                            src = pF[b][:].rearrange("p (q k) -> p q k", q=4)
                            if eng == "act":
                                kb.op("act", lambda dst=dst, src=src: nc.scalar.copy(dst, src), reads=[f"ps:pF{b}"], writes=["Tt"])
                            else:
                                kb.op("dve", lambda dst=dst, src=src: nc.vector.tensor_copy(dst, src), reads=[f"ps:pF{b}"], writes=["Tt"])
                    for c2 in range(2):
                        r0 = c2 * 64
                        for k1 in range(64):
                            b = (k1 // 4) % 2
                            o_ = pG[b][:, (k1 % 4) * 128:(k1 % 4 + 1) * 128]
                            kb.op("pe", lambda o_=o_, r0=r0, k1=k1: nc.tensor.matmul(
                                o_, Tt[r0:r0 + 64, :, k1], tabA[r0:r0 + 64, k1, :], start=True, stop=False),
                                reads=["Tt", "tabA"], writes=[f"ps:pG{b}"], signal=False)
                            kb.op("pe", lambda o_=o_, r0=r0, k1=k1: nc.tensor.matmul(
                                o_, Tt[r0:r0 + 64, :, 64 + k1], tabB[r0:r0 + 64, k1, :], start=False, stop=True),
                                reads=["Tt", "tabB"], writes=[f"ps:pG{b}"], signal=(k1 % 4 == 3))
                            if k1 % 4 == 3:
                                eng = "act" if ev % 2 == 0 else "dve"
                                ev += 1
                                k0 = k1 - 3
                                dst = X[:, c2, :, :].rearrange("p r (j k) -> p r k j", k=64)[:, :, k0:k0 + 4, :]
                                src = pG[b][:].rearrange("p (k r j) -> p r k j", k=4, r=2)
                                if eng == "act":
                                    kb.op("act", lambda dst=dst, src=src: nc.scalar.copy(dst, src), reads=[f"ps:pG{b}"], writes=["X"])
                                else:
                                    kb.op("dve", lambda dst=dst, src=src: nc.vector.tensor_copy(dst, src), reads=[f"ps:pG{b}"], writes=["X"])
                    for ocl in range(2):
                        og = fh * 2 + ocl
                        r0 = 64 * ocl
                        for kc in range(8):
                            b = kc % 2
                            n = 0
                            for c2 in range(2):
                                for ri in range(2):
                                    rhs = X[r0:r0 + 64, c2, ri, kc * 512:(kc + 1) * 512]
                                    kb.op("pe", lambda b=b, rhs=rhs, og=og, c2=c2, ri=ri, r0=r0, n=n: nc.tensor.matmul(
                                        pH[b][:], Mb[r0:r0 + 64, og * 4 + c2 * 2 + ri, :], rhs,
                                        start=(n == 0), stop=(n == 3)),
                                        reads=["X", "Mb"], writes=[f"ps:pH{b}"], signal=(n == 3))
                                    n += 1
                            eng = "act" if ev % 2 == 0 else "dve"
                            ev += 1
                            if eng == "act":
                                kb.op("act", lambda b=b: nc.scalar.copy(yst[b][:], pH[b][:]), reads=[f"ps:pH{b}"], writes=[f"yst{b}"])
                            else:
                                kb.op("dve", lambda b=b: nc.vector.tensor_copy(yst[b][:], pH[b][:]), reads=[f"ps:pH{b}"], writes=[f"yst{b}"])
                            kb.dma_ins("pool", lambda b=b, og=og, kc=kc: nc.gpsimd.indirect_dma_start(
                                out=yT_sh, out_offset=bass.IndirectOffsetOnAxis(ap=yidx[:, og:og + 1], axis=0),
                                in_=yst[b][:], in_offset=None, element_offset=kc * 512),
                                reads=[f"yst{b}", "yidx"], writes=[f"yT_sh{og}_{kc}"], sem=f"ys{b}")
                kb.barrier()
        if upto <= 3:
            if dbg:
                dump("yTsh", yT_sh, [8192, 512], BF16, [f"yT_sh{og}_{kc}" for og in range(2) for kc in range(8)])
            return finish()

        QT = big1[:, 0:16384].rearrange("p (h s) -> p h s", h=4)
        KT = big1[:, 16384:32768].rearrange("p (h s) -> p h s", h=4)
        SCALE = 96.0 ** -0.5
        with ExitStack() as sD:
            V_all = sb(sD, "V_all", [128, T, 2, 192], BF16)
            kb.op("dve", lambda: nc.vector.memset(V_all[:], 0.0), writes=["V_all"])
            kb.op("dve", lambda: nc.vector.memset(V_all[:, :, :, 64:65], 1.0), writes=["V_all"])
            with ExitStack() as sP:
                wqs = sb(sP, "wqs", [128, 3, 512], F32)
                wkvs = sb(sP, "wkvs", [128, 2, 512], F32)
                wq = sb(sP, "wq", [128, 3, 512], BF16)
                wkv = sb(sP, "wkv", [128, 2, 512], BF16)
                gq = sb(sP, "gq", [128, 3], F32)
                gkv = sb(sP, "gkv", [128, 2], F32)
                ut = [sb(sP, f"ut{i}", [128, 6, 128], BF16) for i in range(2)]
                Qt = [sb(sP, f"Qt{i}", [128, 4, 96], BF16) for i in range(2)]
                Kt = [sb(sP, f"Kt{i}", [128, 4, 96], BF16) for i in range(2)]
                cq = [sb(sP, f"cq{i}", [128, 32], F32) for i in range(2)]
                sq_ = [sb(sP, f"sq_{i}", [128, 32], F32) for i in range(2)]
                t1 = [sb(sP, f"t1{i}", [128, 4, 32], F32) for i in range(2)]
                t2 = [sb(sP, f"t2{i}", [128, 4, 32], F32) for i in range(2)]
                kr1 = [sb(sP, f"kr1{i}", [128, 32], F32) for i in range(2)]
                kr2 = [sb(sP, f"kr2{i}", [128, 32], F32) for i in range(2)]
                kro = [sb(sP, f"kro{i}", [128, 32], F32) for i in range(2)]
                pq = [ps(sP, f"pq{i}", [128, 512], F32) for i in range(2)]
                pkv = [ps(sP, f"pkv{i}", [128, 512], F32) for i in range(2)]
                pkr = ps(sP, "pkr", [128, 64], BF16)
                pqT = ps(sP, "pqT", [128, 4, 128], BF16)
                pkT = ps(sP, "pkT", [128, 4, 128], BF16)
                kb.dma("sp", wqs[:], A["w_q"].rearrange("(m p) n -> p m n", p=128), writes=["wqs"], sem="c3a")
                kb.dma("sp", wkvs[:], A["w_kv"].rearrange("(m p) n -> p m n", p=128), writes=["wkvs"], sem="c3b")
                kb.dma("sp", gq[:], A["g_q_lat"], writes=["gq"], sem="c3c")
                kb.dma("sp", gkv[:], A["g_kv_lat"], writes=["gkv"], sem="c3d")
                for m in range(3):
                    kb.op("pool", lambda m=m: nc.gpsimd.tensor_scalar(wq[:, m, :], wqs[:, m, :], gq[:, m:m + 1], None, ALU.mult),
                          reads=["wqs", "gq"], writes=["wq"])
                for m in range(2):
                    kb.op("pool", lambda m=m: nc.gpsimd.tensor_scalar(wkv[:, m, :], wkvs[:, m, :], gkv[:, m:m + 1], None, ALU.mult),
                          reads=["wkvs", "gkv"], writes=["wkv"])
                for t in range(T):
                    i = t % 2
                    tl = slice(t * 128, (t + 1) * 128)
                    kb.dma("sp", ut[i][:, 0:5, :], uT_d[0:5, :, tl].rearrange("m p s -> p m s"),
                           reads=["uT_d"], writes=[f"ut{i}"], sem=f"ut{i}")
                    kb.dma("sp", ut[i][0:64, 5, :], uT_d[5, 0:64, tl], reads=["uT_d"], writes=[f"ut{i}"], sem=f"ut{i}")
                    for m in range(3):
                        kb.op("pe", lambda m=m, i=i: nc.tensor.matmul(
                            pq[i][:], ut[i][:, m, :], wq[:, m, :], start=(m == 0), stop=(m == 2)),
                            reads=[f"ut{i}", "wq"], writes=[f"ps:pq{i}"], signal=(m == 2))
                    for m in range(2):
                        kb.op("pe", lambda m=m, i=i: nc.tensor.matmul(
                            pkv[i][:], ut[i][:, 3 + m, :], wkv[:, m, :], start=(m == 0), stop=(m == 1)),
                            reads=[f"ut{i}", "wkv"], writes=[f"ps:pkv{i}"], signal=(m == 1))
                    kb.op("pe", lambda i=i: nc.tensor.transpose(pkr[:], ut[i][0:64, 5, :], ident_bf[0:64, 0:64]),
                          reads=[f"ut{i}", "ident_bf"], writes=["ps:pkr"])
                    qv = pq[i][:, 0:384].rearrange("p (h d) -> p h d", d=96)
                    qs = pq[i][:, 384:512].rearrange("p (h d) -> p h d", d=32)
                    kb.op("act", lambda qv=qv, i=i, t=t: nc.scalar.activation(
                        Qt[i][:, :, 0:64], qv[:, :, 0:64], AF.Copy, scale=rq[:, t:t + 1]),
                        reads=[f"ps:pq{i}", "rq"], writes=[f"Qt{i}"])
                    kb.op("dve", lambda qv=qv, i=i, t=t: nc.vector.tensor_tensor(
                        t1[i][:], qv[:, :, 64:96], cos2[:, t, :].unsqueeze(1).to_broadcast([128, 4, 32]), ALU.mult),
                        reads=[f"ps:pq{i}", "rope_tab"], writes=[f"t1{i}"])
                    kb.op("dve", lambda qs=qs, i=i, t=t: nc.vector.scalar_tensor_tensor(
                        t2[i][:], qs, rq[:, t:t + 1], sin2s[:, t, :].unsqueeze(1).to_broadcast([128, 4, 32]), ALU.mult, ALU.mult),
                        reads=[f"ps:pq{i}", "rope_tab", "rq"], writes=[f"t2{i}"])
                    kb.op("dve", lambda i=i, t=t: nc.vector.scalar_tensor_tensor(
                        Qt[i][:, :, 64:96], t1[i][:], rq[:, t:t + 1], t2[i][:], ALU.mult, ALU.add),
                        reads=[f"t1{i}", f"t2{i}", "rq"], writes=[f"Qt{i}"])
                    kb.op("act", lambda i=i, t=t: nc.scalar.activation(
                        Kt[i][:, :, 0:64], pkv[i][:, 0:256].rearrange("p (h d) -> p h d", d=64), AF.Copy, scale=rkv[:, t:t + 1]),
                        reads=[f"ps:pkv{i}", "rkv"], writes=[f"Kt{i}"])
                    kb.op("dve", lambda t=t, i=i: nc.vector.tensor_tensor(kr1[i][:], pkr[:, 0:32], cos2[:, t, :], ALU.mult),
                          reads=["ps:pkr", "rope_tab"], writes=[f"kr1{i}"])
                    kb.op("dve", lambda t=t, i=i: nc.vector.tensor_tensor(kr2[i][:], pkr[:, 32:64], sin2s[:, t, :], ALU.mult),
                          reads=["ps:pkr", "rope_tab"], writes=[f"kr2{i}"])
                    kb.op("pool", lambda i=i: nc.gpsimd.tensor_tensor(kro[i][:], kr1[i][:], kr2[i][:], ALU.add),
                          reads=[f"kr1{i}", f"kr2{i}"], writes=[f"kro{i}"])
                    kb.op("pool", lambda i=i: nc.gpsimd.tensor_copy(
                        Kt[i][:, :, 64:96], kro[i][:].unsqueeze(1).to_broadcast([128, 4, 32])),
                        reads=[f"kro{i}"], writes=[f"Kt{i}"])
                    vv4 = pkv[i][:, 256:512].rearrange("p (a b d) -> p a b d", a=2, b=2)
                    kb.op("act", lambda t=t, vv4=vv4: nc.scalar.activation(
                        V_all[:, t, :, 0:64], vv4[:, :, 0, :], AF.Copy, scale=rkv[:, t:t + 1]),
                        reads=[f"ps:pkv{i}", "rkv"], writes=["V_all"])
                    kb.op("dve", lambda t=t, vv4=vv4: nc.vector.tensor_scalar(
                        V_all[:, t, :, 128:192], vv4[:, :, 1, :], rkv[:, t:t + 1], None, ALU.mult),
                        reads=[f"ps:pkv{i}", "rkv"], writes=["V_all"])
                    for h in range(4):
                        kb.op("pe", lambda h=h, i=i: nc.tensor.transpose(pqT[0:96, h, :], Qt[i][:, h, :], ident_bf[:]),
                              reads=[f"Qt{i}", "ident_bf"], writes=["ps:pqT"], signal=(h == 3))
                    for h in range(4):
                        kb.op("pe", lambda h=h, i=i: nc.tensor.transpose(pkT[0:96, h, :], Kt[i][:, h, :], ident_bf[:]),
                              reads=[f"Kt{i}", "ident_bf"], writes=["ps:pkT"], signal=(h == 3))
                    kb.op("act", lambda tl=tl: nc.scalar.copy(QT[0:96, :, tl], pqT[0:96, :, :]), reads=["ps:pqT"], writes=["QT"])
                    kb.op("dve", lambda tl=tl: nc.vector.tensor_copy(KT[0:96, :, tl], pkT[0:96, :, :]), reads=["ps:pkT"], writes=["KT"])
                kb.barrier()
            if upto <= 4:
                if dbg:
                    dump("QT", big1[0:96, 0:16384], [96, 16384], BF16, ["QT"])
                    dump("KT", big1[0:96, 16384:32768], [96, 16384], BF16, ["KT"])
                    dump("V", V_all[:], [128, T, 2, 192], BF16, ["V_all"])
                return finish()
            with ExitStack() as sM:
                NB = 4
                PT = [sb(sM, f"PT{i}", [128, 512], BF16) for i in range(NB)]
                recs = sb(sM, "recs", [128, 512], F32)
                bcs = sb(sM, "bcs", [128, 512], F32)
                ysa = [sb(sM, f"ysa{i}", [128, 512], BF16) for i in range(2)]
                pS = [ps(sM, f"pS{i}", [128, 512], F32) for i in range(NB)]
                pO = [ps(sM, f"pO{i}", [128, 512], F32) for i in range(2)]
                pbc = ps(sM, "pbc", [128, 512], F32)
                ngroups = 1
                nqc = int(os.environ.get("KNQC", "8"))
                yi = 0
                for g2 in range(ngroups):
                    if g2 == 1:
                        kb.barrier()
                        kb.dma("sp", QT[0:96, :, :], qk_d[0], reads=["qk_d"], writes=["QT"], sem="c4")
                        kb.dma("sp", KT[0:96, :, :], qk_d[1], reads=["qk_d"], writes=["KT"], sem="c4")
                    for pair in range(2):
                        gp = 2 * g2 + pair
                        for qc in range(nqc):
                            qsl = slice(qc * 512, (qc + 1) * 512)
                            yb = yi % 2
                            yi += 1
                            for hh in range(2):
                                hl = 2 * pair + hh
                                O = pO[hh]
                                if hh == 0:
                                    lv = lambda kt: V_all[:, kt, gp, 0:65]
                                    Mo = 65
                                else:
                                    lv = lambda kt: V_all[:, kt, gp, 64:192]
                                    Mo = 128

                                def smm(kt, hl=hl, qsl=qsl):
                                    b = kt % NB
                                    kb.op("pe", lambda b=b, kt=kt: nc.tensor.matmul(
                                        pS[b][:], KT[0:96, hl, kt * 128:(kt + 1) * 128], QT[0:96, hl, qsl],
                                        start=True, stop=True),
                                        reads=["QT", "KT"], writes=[f"ps:pS{b}"])
                                smm(0)
                                smm(1)
                                for kt in range(T):
                                    b = kt % NB
                                    if kt + 2 < T:
                                        smm(kt + 2)
                                    kb.op("act", lambda b=b: nc.scalar.activation(PT[b][:], pS[b][:], AF.Exp, scale=SCALE),
                                          reads=[f"ps:pS{b}"], writes=[f"PT{b}"])
                                    kb.op("pe", lambda b=b, kt=kt, O=O, lv=lv, Mo=Mo: nc.tensor.matmul(
                                        O[0:Mo, :], lv(kt), PT[b][:], start=(kt == 0), stop=(kt == T - 1)),
                                        reads=[f"PT{b}", "V_all"], writes=[f"ps:pO{hh}"], signal=(kt == T - 1))
                                if hh == 0:
                                    kb.op("dve", lambda O=O: nc.vector.reciprocal(recs[64:65, :], O[64:65, :]),
                                          reads=["ps:pO0"], writes=["recs"])
                                    kb.op("pe", lambda: nc.tensor.matmul(pbc[0:64, :], ones_f[64:65, 0:64], recs[64:65, :],
                                                                         start=True, stop=True),
                                          reads=["recs", "ones_f"], writes=["ps:pbc"])
                                    kb.op("act", lambda: nc.scalar.copy(bcs[0:64, :], pbc[0:64, :]), reads=["ps:pbc"], writes=["bcs"])
                                    kb.op("dve", lambda O=O, yb=yb: nc.vector.tensor_tensor(ysa[yb][0:64, :], O[0:64, :], bcs[0:64, :], ALU.mult),
                                          reads=["ps:pO0", "bcs"], writes=[f"ysa{yb}"])
                                else:
                                    kb.op("dve", lambda O=O: nc.vector.reciprocal(recs[0:1, :], O[0:1, :]),
                                          reads=["ps:pO1"], writes=["recs"])
                                    kb.op("pe", lambda: nc.tensor.matmul(pbc[:, :], ones_f[0:1, :], recs[0:1, :],
                                                                         start=True, stop=True),
                                          reads=["recs", "ones_f"], writes=["ps:pbc"])
                                    kb.op("act", lambda: nc.scalar.copy(bcs[64:128, :], pbc[64:128, :]), reads=["ps:pbc"], writes=["bcs"])
                                    kb.op("dve", lambda O=O, yb=yb: nc.vector.tensor_tensor(ysa[yb][64:128, :], O[64:128, :], bcs[64:128, :], ALU.mult),
                                          reads=["ps:pO1", "bcs"], writes=[f"ysa{yb}"])
                            kb.dma_ins("pool", lambda yb=yb, gp=gp, qc=qc: nc.gpsimd.indirect_dma_start(
                                out=yT_sh, out_offset=bass.IndirectOffsetOnAxis(ap=yidx[:, 2 + gp:3 + gp], axis=0),
                                in_=ysa[yb][:], in_offset=None, element_offset=qc * 512),
                                reads=[f"ysa{yb}", "yidx"], writes=[f"yT_sha{gp}_{qc}"], sem=f"ya{yb}")
                kb.barrier()
        if upto <= 5:
            if dbg:
                dump("yTsh", yT_sh, [8192, 512], BF16, ["uT_d"])
            return finish()

        sAD.close()
        pair_barrier(0)
        h3rows = big1[:, 0:HT * ROW].rearrange("p (t r) -> p t r", t=HT)

        def rstd_of(col, t, n):
            kb.op("act", lambda: nc.scalar.activation(rsall[:, col, t:t + 1], ssall[:, col, t:t + 1], AF.Sqrt,
                                                      bias=EPS, scale=1.0 / n),
                  reads=[f"ss{col}_{t}"], writes=[f"rs{col}_{t}"])
            kb.op("dve", lambda: nc.vector.reciprocal(rsall[:, col, t:t + 1], rsall[:, col, t:t + 1]),
                  reads=[f"rs{col}_{t}"], writes=[f"rs{col}_{t}"])

        with ExitStack() as sE:
            wo = sb(sE, "wo", [128, 8, D], BF16)
            wmq = sb(sE, "wmq", [128, 8, D], BF16)
            wmo = sb(sE, "wmo", [128, 8, D], BF16)
            wr = sb(sE, "wr", [128, 8, NE], F32)
            gmq = sb(sE, "gmq", [128, D], F32)
            gffn = sb(sE, "gffn", [128, D], F32)
            tokid = sb(sE, "tokid", [128, HT], I32)
            gidx = sb(sE, "gidx", [128, 8], I32)
            x2idx = sb(sE, "x2idx", [128, HT], I32)
            zidx = sb(sE, "zidx", [128, HT], I32)
            hidx = sb(sE, "hidx", [128, 1], I32)
            aidx = sb(sE, "aidx", [128, 2], I32)
            zt = sb(sE, "zt", [128, D], F32)
            for nm_, t_ in (("gidx", gidx), ("x2idx", x2idx), ("zidx", zidx), ("hidx", hidx), ("aidx", aidx)):
                kb.dma("sp", t_[:], A[nm_], writes=[nm_], sem="c5")
            kb.join(["gidx", "x2idx", "zidx", "hidx", "aidx"], "c5")
            kb.op("pool", lambda: nc.gpsimd.memset(zt[:], 0.0), writes=["zt"])
            for tl_ in range(HT):
                kb.dma_ins("pool", lambda tl_=tl_: nc.gpsimd.indirect_dma_start(
                    out=accp_sh, out_offset=bass.IndirectOffsetOnAxis(ap=zidx[:, tl_:tl_ + 1], axis=0),
                    in_=zt[:], in_offset=None), reads=["zt", "zidx"], writes=[f"accz{tl_}"], sem="az")
            kmT = sb(sE, "kmT", [128, 8, 256], BF16)
            vm = sb(sE, "vm", [128, 2, D], BF16)
            for c in range(8):
                cs = slice(c * 128, (c + 1) * 128)
                kb.dma("pool", wo[:, c, :], A["w_out"][cs, :], writes=[f"wo_{c}"], sem="we")
                kb.dma("pool", wmq[:, c, :], A["w_mem_q"][cs, :], writes=[f"wmq_{c}"], sem="we")
                kb.dma("pool", wmo[:, c, :], A["w_mem_o"][cs, :], writes=[f"wmo_{c}"], sem="we")
            kb.join(["wo", "wmq", "wmo"], "we")
            kb.dma("sp", wr[:], A["w_router"].rearrange("(c p) e -> p c e", p=128), writes=["wr"], sem="c5b")
            kb.dma("sp", gmq[:], A["g_mem_q"], writes=["gmq"], sem="c5b")
            kb.dma("sp", gffn[:], A["g_ffn"], writes=["gffn"], sem="c5b")
            kb.dma("sp", tokid[:], A["tokid"], writes=["tokid"], sem="c5b")
            kb.join(["wr", "gmq", "gffn", "tokid"], "c5b")
            kb.op("pool", lambda: nc.gpsimd.memset(big1[:, 0:HT * ROW], 0.0), writes=["h3rows"])
            kb.op("pool", lambda: nc.gpsimd.tensor_copy(
                h3rows[:, :, 1056:1058].bitcast(I32), tokid[:].unsqueeze(2)), reads=["tokid"], writes=["h3rows"])
            with ExitStack() as sK:
                wmkv = sb(sK, "wmkv", [128, 8, 2048], BF16)
                gmkv = sb(sK, "gmkv", [128, D], F32)
                mt_ = sb(sK, "mt_", [128, D], F32)
                mb_ = sb(sK, "mb_", [128, D], BF16)
                sqm = sb(sK, "sqm", [128, D], BF16)
                mnT = sb(sK, "mnT", [128, 8, 256], BF16)
                pT = ps(sK, "pTm", [128, 8, 128], BF16)
                pA = [ps(sK, f"pAm{i}", [128, 512], F32) for i in range(2)]
                for c in range(8):
                    kb.dma("pool", wmkv[:, c, :], A["w_mem_kv"][c * 128:(c + 1) * 128, :], writes=[f"wmkv_{c}"], sem="wk")
                kb.join(["wmkv"], "wk")
                kb.dma("sp", gmkv[:], A["g_mem_kv"], writes=["gmkv"], sem="c5g")
                for mt in range(2):
                    kb.dma("sp", mt_[:], A["mem"][mt * 128:(mt + 1) * 128, :], writes=["mt_"], sem="c5m")
                    kb.op("act", lambda mt=mt: nc.scalar.activation(sqm[:], mt_[:], AF.Square, accum_out=ssall[:, 4, mt:mt + 1]),
                          reads=["mt_"], writes=["sqm", f"ss4_{mt}"])
                    rstd_of(4, mt, D)
                    kb.op("dve", lambda mt=mt: nc.vector.scalar_tensor_tensor(
                        mb_[:], mt_[:], rsall[:, 4, mt:mt + 1], gmkv[:], ALU.mult, ALU.mult),
                        reads=["mt_", f"rs4_{mt}", "gmkv"], writes=["mb_"])
                    for c in range(8):
                        kb.op("pe", lambda c=c: nc.tensor.transpose(pT[:, c, :], mb_[:, c * 128:(c + 1) * 128], ident_bf[:]),
                              reads=["mb_", "ident_bf"], writes=["ps:pTm"], signal=(c == 7))
                    kb.op("act", lambda mt=mt: nc.scalar.copy(mnT[:, :, mt * 128:(mt + 1) * 128], pT[:]),
                          reads=["ps:pTm"], writes=["mnT"])
                n_ = 0
                for hc in range(8):
                    b = n_ % 2
                    n_ += 1
                    for c in range(8):
                        kb.op("pe", lambda b=b, hc=hc, c=c: nc.tensor.matmul(
                            pA[b][:, 0:256], wmkv[:, c, hc * 128:(hc + 1) * 128], mnT[:, c, :], start=(c == 0), stop=(c == 7)),
                            reads=["wmkv", "mnT"], writes=[f"ps:pAm{b}"], signal=(c == 7))
                    kb.op("dve", lambda b=b, hc=hc: nc.vector.tensor_copy(kmT[:, hc, :], pA[b][:, 0:256]),
                          reads=[f"ps:pAm{b}"], writes=["kmT"])
                for mt in range(2):
                    for half in range(2):
                        b = n_ % 2
                        n_ += 1
                        for c in range(8):
                            kb.op("pe", lambda b=b, mt=mt, half=half, c=c: nc.tensor.matmul(
                                pA[b][:], mnT[:, c, mt * 128:(mt + 1) * 128],
                                wmkv[:, c, 1024 + half * 512:1024 + (half + 1) * 512], start=(c == 0), stop=(c == 7)),
                                reads=["wmkv", "mnT"], writes=[f"ps:pAm{b}"], signal=(c == 7))
                        kb.op("act", lambda b=b, mt=mt, half=half: nc.scalar.copy(vm[:, mt, half * 512:(half + 1) * 512], pA[b][:]),
                              reads=[f"ps:pAm{b}"], writes=["vm"])
                kb.barrier()
            with ExitStack() as sL:
                UP = 17408
                yt = [sb(sL, "yt0", [128, 8, 512], BF16), big1[:, UP:UP + 4096].rearrange("p (c s) -> p c s", c=8)]
                qmT = big1[:, UP + 4096:UP + 8192].rearrange("p (c s) -> p c s", c=8)
                OT = big1[:, UP + 8192:UP + 12288].rearrange("p (c s) -> p c s", c=8)
                xt = [sb(sL, f"xe{i}", [128, D], F32) for i in range(2)]
                x1s = sb(sL, "x1s", [128, 4, D], F32)
                hb = [sb(sL, f"hbe{i}", [128, D], BF16) for i in range(2)]
                sqj = sb(sL, "sqe", [128, D], BF16)
                h2T = sb(sL, "h2T", [128, 8, 512], BF16)
                PTm = [sb(sL, f"PTm{i}", [128, 512], BF16) for i in range(2)]
                recm = sb(sL, "recm", [128, 512], F32)
                h3f = [sb(sL, f"h3f{i}", [128, D], F32) for i in range(2)]
                h3T = sb(sL, "h3T", [128, 8, 128], F32)
                lg = sb(sL, "lg", [128, NE], F32)
                ex = sb(sL, "ex", [128, NE], F32)
                sm = sb(sL, "sm", [128, 4], F32)
                pX = [ps(sL, f"pX{i}", [128, 512], F32) for i in range(2)]
                pT = ps(sL, "pTe", [128, 8, 128], BF16)
                pA = [ps(sL, f"pAe{i}", [128, 512], F32) for i in range(2)]
                pO = [ps(sL, f"pOe{i}", [128, 512], F32) for i in range(2)]
                pSm = ps(sL, "pSm", [128, 512], F32)
                MSC = 256.0 ** -0.5
                nst = 4

                def load_yt(n):
                    yb = n % 2
                    war = list(kb.readers.get(f"yt{yb}", []))
                    tok = None
                    for c in range(8):
                        tok = kb.dma_ins("pool", lambda c=c, n=n, yb=yb: nc.gpsimd.indirect_dma_start(
                            out=yt[yb][:, c, :], out_offset=None, in_=yT_sh,
                            in_offset=bass.IndirectOffsetOnAxis(ap=gidx[:, c:c + 1], axis=0), element_offset=n * 512),
                            reads=["gidx"], writes=[], sem=f"yt{yb}", extra=war)
                    kb.lastw[f"yt{yb}"] = tok
                    kb.readers[f"yt{yb}"] = []

                def s1_front(n, j):
                    t = 4 * n + j
                    i = t % 2
                    yb = n % 2
                    js = slice(j * 128, (j + 1) * 128)
                    kb.dma("sp", xt[i][:], A["xh"][t * 128:(t + 1) * 128, :], writes=[f"xe{i}"], sem=f"xe{i}")
                    for half in range(2):
                        hs = slice(half * 512, (half + 1) * 512)
                        for c in range(8):
                            kb.op("pe", lambda half=half, c=c, js=js, hs=hs, yb=yb: nc.tensor.matmul(
                                pX[half][:], yt[yb][:, c, js], wo[:, c, hs], start=(c == 0), stop=(c == 7)),
                                reads=[f"yt{yb}", "wo"], writes=[f"ps:pX{half}"], signal=(c == 7))
                        kb.op("dve", lambda half=half, j=j, hs=hs, i=i: nc.vector.tensor_tensor(
                            x1s[:, j, hs], pX[half][:], xt[i][:, hs], ALU.add),
                            reads=[f"ps:pX{half}", f"xe{i}"], writes=[f"x1s{j}"])
                    kb.op("act", lambda j=j, t=t: nc.scalar.activation(sqj[:], x1s[:, j, :], AF.Square,
                                                                       accum_out=ssall[:, 1, t:t + 1]),
                          reads=[f"x1s{j}"], writes=["sqe", f"ss1_{t}"])
                    rstd_of(1, t, D)
                    kb.op("dve", lambda j=j, t=t, i=i: nc.vector.scalar_tensor_tensor(
                        hb[i][:], x1s[:, j, :], rsall[:, 1, t:t + 1], gmq[:], ALU.mult, ALU.mult),
                        reads=[f"x1s{j}", f"rs1_{t}", "gmq"], writes=[f"hbe{i}"])

                def s1_back(n, j):
                    t = 4 * n + j
                    i = t % 2
                    js = slice(j * 128, (j + 1) * 128)
                    for c in range(8):
                        kb.op("pe", lambda c=c, i=i: nc.tensor.transpose(pT[:, c, :], hb[i][:, c * 128:(c + 1) * 128], ident_bf[:]),
                              reads=[f"hbe{i}", "ident_bf"], writes=["ps:pTe"], signal=(c == 7))
                    if j % 2 == 0:
                        kb.op("act", lambda js=js: nc.scalar.copy(h2T[:, :, js], pT[:]), reads=["ps:pTe"], writes=["h2T"])
                    else:
                        kb.op("dve", lambda js=js: nc.vector.tensor_copy(h2T[:, :, js], pT[:]), reads=["ps:pTe"], writes=["h2T"])

                def s3_front(n, j):
                    t = 4 * n + j
                    i = j
                    k = t % 2
                    js = slice(j * 128, (j + 1) * 128)
                    for half in range(2):
                        hs = slice(half * 512, (half + 1) * 512)
                        for c in range(8):
                            kb.op("pe", lambda half=half, c=c, js=js, hs=hs: nc.tensor.matmul(
                                pX[half][:], OT[:, c, js], wmo[:, c, hs], start=(c == 0), stop=(c == 7)),
                                reads=["OT", "wmo"], writes=[f"ps:pX{half}"], signal=(c == 7))
                        kb.op("dve", lambda half=half, j=j, hs=hs: nc.vector.tensor_tensor(
                            x1s[:, j, hs], pX[half][:], x1s[:, j, hs], ALU.add),
                            reads=[f"ps:pX{half}", f"x1s{j}"], writes=[f"x1s{j}"])
                    kb.dma_ins("pool", lambda t=t, j=j: nc.gpsimd.indirect_dma_start(
                        out=accp_sh, out_offset=bass.IndirectOffsetOnAxis(ap=x2idx[:, t:t + 1], axis=0),
                        in_=x1s[:, j, :], in_offset=None), reads=[f"x1s{j}", "x2idx"], writes=[f"accx{t}"], sem=f"x2{i}")
                    kb.op("act", lambda j=j, t=t: nc.scalar.activation(sqj[:], x1s[:, j, :], AF.Square,
                                                                       accum_out=ssall[:, 2, t:t + 1]),
                          reads=[f"x1s{j}"], writes=["sqe", f"ss2_{t}"])
                    rstd_of(2, t, D)
                    kb.op("dve", lambda j=j, t=t, k=k: nc.vector.scalar_tensor_tensor(
                        h3f[k][:], x1s[:, j, :], rsall[:, 2, t:t + 1], gffn[:], ALU.mult, ALU.mult),
                        reads=[f"x1s{j}", f"rs2_{t}", "gffn"], writes=[f"h3f{k}"])
                    kb.op("pool", lambda t=t, k=k: nc.gpsimd.tensor_copy(h3rows[:, t, 0:D], h3f[k][:]),
                          reads=[f"h3f{k}"], writes=["h3rows"])

                def s3_back(n, j):
                    t = 4 * n + j
                    k = t % 2
                    for half in range(2):
                        pXv = pA[half][:].rearrange("p (c k) -> p c k", c=4)
                        for c4 in range(4):
                            c = half * 4 + c4
                            kb.op("pe", lambda pXv=pXv, c4=c4, c=c, k=k: nc.tensor.transpose(
                                pXv[:, c4, :], h3f[k][:, c * 128:(c + 1) * 128], ident_f[:]),
                                reads=[f"h3f{k}", "ident_f"], writes=[f"ps:pAe{half}"], signal=(c4 == 3))
                        if half == 0:
                            kb.op("act", lambda pXv=pXv: nc.scalar.copy(h3T[:, 0:4, :], pXv), reads=["ps:pAe0"], writes=["h3T"])
                        else:
                            kb.op("dve", lambda pXv=pXv: nc.vector.tensor_copy(h3T[:, 4:8, :], pXv), reads=["ps:pAe1"], writes=["h3T"])
                    for c in range(8):
                        kb.op("pe", lambda c=c: nc.tensor.matmul(pSm[:, 0:NE], h3T[:, c, :], wr[:, c, :], start=(c == 0), stop=(c == 7)),
                              reads=["h3T", "wr"], writes=["ps:pSm"], signal=(c == 7))
                    kb.op("dve", lambda: nc.vector.tensor_copy(lg[:], pSm[:, 0:NE]), reads=["ps:pSm"], writes=["lg"])
                    kb.op("dve", lambda: nc.vector.tensor_reduce(sm[:, 0:1], lg[:], AX.X, ALU.max), reads=["lg"], writes=["sm0"])
                    kb.op("dve", lambda: nc.vector.tensor_scalar(sm[:, 1:2], sm[:, 0:1], -1.0, None, ALU.mult), reads=["sm0"], writes=["sm1"])
                    kb.op("act", lambda t=t: nc.scalar.activation(ex[:], lg[:], AF.Exp, bias=sm[:, 1:2], accum_out=ssall[:, 3, t:t + 1]),
                          reads=["lg", "sm1"], writes=["ex", f"ss3_{t}"])
                    kb.op("dve", lambda t=t: nc.vector.reciprocal(sm[:, 2:3], ssall[:, 3, t:t + 1]), reads=[f"ss3_{t}"], writes=["sm2"])
                    kb.op("dve", lambda t=t: nc.vector.tensor_scalar(affl[:, t, :], ex[:], sm[:, 2:3], None, ALU.mult),
                          reads=["ex", "sm2"], writes=["affl"])
                    kb.op("pool", lambda t=t: nc.gpsimd.tensor_copy(h3rows[:, t, 1024:1056].bitcast(F32), affl[:, t, :]),
                          reads=["affl"], writes=["h3rows"])

                load_yt(0)
                for n in range(nst):
                    s1_front(n, 0)
                    for j in range(4):
                        if j + 1 < 4:
                            s1_front(n, j + 1)
                        s1_back(n, j)
                    if n + 1 < nst:
                        load_yt(n + 1)
                    for hc in range(8):
                        b = hc % 2
                        for c in range(8):
                            kb.op("pe", lambda b=b, hc=hc, c=c: nc.tensor.matmul(
                                pA[b][:], wmq[:, c, hc * 128:(hc + 1) * 128], h2T[:, c, :], start=(c == 0), stop=(c == 7)),
                                reads=["wmq", "h2T"], writes=[f"ps:pAe{b}"], signal=(c == 7))
                        if hc % 2 == 0:
                            kb.op("act", lambda b=b, hc=hc: nc.scalar.copy(qmT[:, hc, :], pA[b][:]), reads=[f"ps:pAe{b}"], writes=["qmT"])
                        else:
                            kb.op("dve", lambda b=b, hc=hc: nc.vector.tensor_copy(qmT[:, hc, :], pA[b][:]), reads=[f"ps:pAe{b}"], writes=["qmT"])
                    for hh in range(4):
                        for mt in range(2):
                            for dc in range(2):
                                kb.op("pe", lambda mt=mt, dc=dc, hh=hh: nc.tensor.matmul(
                                    pA[mt][0:128, :], kmT[:, hh * 2 + dc, mt * 128:(mt + 1) * 128], qmT[:, hh * 2 + dc, :],
                                    start=(dc == 0), stop=(dc == 1)),
                                    reads=["kmT", "qmT"], writes=[f"ps:pAe{mt}"], signal=(dc == 1))
                            kb.op("act", lambda mt=mt: nc.scalar.activation(PTm[mt][:], pA[mt][:], AF.Exp, scale=MSC),
                                  reads=[f"ps:pAe{mt}"], writes=[f"PTm{mt}"])
                        for mt in range(2):
                            kb.op("pe", lambda mt=mt: nc.tensor.matmul(pSm[:], ones_bf[:], PTm[mt][:], start=(mt == 0), stop=(mt == 1)),
                                  reads=["ones_bf", f"PTm{mt}"], writes=["ps:pSm"], signal=(mt == 1))
                        for dc in range(2):
                            for mt in range(2):
                                kb.op("pe", lambda mt=mt, dc=dc, hh=hh: nc.tensor.matmul(
                                    pO[dc][:], vm[:, mt, hh * 256 + dc * 128:hh * 256 + (dc + 1) * 128], PTm[mt][:],
                                    start=(mt == 0), stop=(mt == 1)),
                                    reads=["vm", f"PTm{mt}"], writes=[f"ps:pOe{dc}"], signal=(mt == 1))
                        kb.op("dve", lambda: nc.vector.reciprocal(recm[:], pSm[:]), reads=["ps:pSm"], writes=["recm"])
                        for dc in range(2):
                            kb.op("dve", lambda dc=dc, hh=hh: nc.vector.tensor_tensor(OT[:, hh * 2 + dc, :], pO[dc][:], recm[:], ALU.mult),
                                  reads=[f"ps:pOe{dc}", "recm"], writes=["OT"])
                    s3_front(n, 0)
                    for j in range(4):
                        if j + 1 < 4:
                            s3_front(n, j + 1)
                        s3_back(n, j)
                kb.dma_ins("pool", lambda: nc.gpsimd.indirect_dma_start(
                    out=affs_sh, out_offset=bass.IndirectOffsetOnAxis(ap=hidx[:, 0:1], axis=0),
                    in_=affl[:].rearrange("p t e -> p (t e)"), in_offset=None),
                    reads=["affl", "hidx"], writes=["affs_sh"], sem="afs")
                pair_barrier(1)
                for hh_ in range(2):
                    kb.dma_ins("pool", lambda hh_=hh_: nc.gpsimd.indirect_dma_start(
                        out=affs[:, hh_ * HT:(hh_ + 1) * HT, :].rearrange("p t e -> p (t e)"), out_offset=None, in_=affs_sh,
                        in_offset=bass.IndirectOffsetOnAxis(ap=aidx[:, hh_:hh_ + 1], axis=0)),
                        reads=["aidx", "affs_sh"], writes=["affs"], sem="afg")
                kb.barrier()
        if upto <= 6:
            if dbg:
                dump("affs", affs[:], [128, T, NE], F32, ["affs"])
                dump("h3rows", big1[:, 0:HT * ROW], [128, HT * ROW], BF16, ["h3rows"])
                dump("accp", accp_sh, [2 * S, D], F32, ["affs"])
            return finish()

        with ExitStack() as sT:
            lo = sb(sT, "lo", [128, NE], F32)
            mid = sb(sT, "mid", [128, NE], F32)
            dlt = sb(sT, "dlt", [128, NE], F32)
            cnt = sb(sT, "cnt", [128, NE], F32)
            msk = sb(sT, "msk", [128, T, NE], F32)
            sel = sb(sT, "sel", [128, T, NE], BF16)
            tot = sb(sT, "tot", [128, T, NE], F32)
            incl = sb(sT, "incl", [128, NE, HT], F32)
            posf = sb(sT, "posf2", [128, HT, NE], F32)
            pen = sb(sT, "pen", [128, HT, NE], F32)
            toth = sb(sT, "toth", [128, NE], F32)
            eoff = sb(sT, "eoff", [128, HT, NE], F32)
            kb.dma("sp", eoff[:], A["eoff"], writes=["eoff"], sem="c7")
            pC = ps(sT, "pC", [128, 512], F32)
            pP = ps(sT, "pP", [128, 512], F32)
            pQ = ps(sT, "pQ", [128, 512], F32)
            kb.op("dve", lambda: nc.vector.memset(lo[:], 0.0), writes=["lo"])
            for k in range(34):
                w2 = 1.5 / (2.0 ** (k + 1))
                kb.op("dve", lambda w2=w2: nc.vector.tensor_scalar(mid[:], lo[:], w2, None, ALU.add), reads=["lo"], writes=["mid"])
                kb.op("dve", lambda: nc.vector.tensor_tensor(
                    msk[:], affs[:], mid[:].unsqueeze(1).to_broadcast([128, T, NE]), ALU.is_ge),
                    reads=["affs", "mid"], writes=["msk"])
                kb.op("dve", lambda: nc.vector.tensor_reduce(cnt[:], msk[:].rearrange("p t e -> p e t"), AX.X, ALU.add),
                      reads=["msk"], writes=["cnt"])
                kb.op("pe", lambda: nc.tensor.matmul(pC[:, 0:NE], ones_f[:], cnt[:], start=True, stop=True),
                      reads=["ones_f", "cnt"], writes=["ps:pC"])
                kb.op("dve", lambda w2=w2: nc.vector.tensor_scalar(dlt[:], pC[:, 0:NE], CAP - 0.5, w2, ALU.is_ge, ALU.mult),
                      reads=["ps:pC"], writes=["dlt"])
                kb.op("dve", lambda: nc.vector.tensor_tensor(lo[:], lo[:], dlt[:], ALU.add), reads=["lo", "dlt"], writes=["lo"])
            kb.op("dve", lambda: nc.vector.tensor_tensor(
                sel[:], affs[:], lo[:].unsqueeze(1).to_broadcast([128, T, NE]), ALU.is_ge),
                reads=["affs", "lo"], writes=["sel"])
            self_flat = sel[:].rearrange("p t e -> p (t e)")
            own_flat = sel[:, 0:HT, :].rearrange("p t e -> p (t e)")
            kb.op("pe", lambda: nc.tensor.matmul(pP[:, 0:HT * NE], ustrict[:], own_flat, start=True, stop=True),
                  reads=["ustrict", "sel"], writes=["ps:pP"])
            kb.op("pe", lambda: nc.tensor.matmul(pQ[:], ones_bf[:], self_flat, start=True, stop=True),
                  reads=["ones_bf", "sel"], writes=["ps:pQ"])
            kb.op("dve", lambda: nc.vector.tensor_copy(tot[:].rearrange("p t e -> p (t e)"), pQ[:]), reads=["ps:pQ"], writes=["tot"])
            for e in range(NE):
                kb.op("dve", lambda e=e: nc.vector.tensor_tensor_scan(
                    incl[:, e, :], ones_f[:, 0:HT], tot[:, 0:HT, e], 0.0, ALU.mult, ALU.add),
                    reads=["tot", "ones_f"], writes=["incl"])
            kb.op("dve", lambda: nc.vector.tensor_reduce(toth[:], tot[:, HT:T, :].rearrange("p t e -> p e t"), AX.X, ALU.add),
                  reads=["tot"], writes=["toth"])
            kb.op("dve", lambda: nc.vector.tensor_scalar(toth[:], toth[:], jf[:, 0:1], None, ALU.mult), reads=["toth", "jf"], writes=["toth"])
            kb.op("dve", lambda: nc.vector.tensor_tensor(posf[:], incl[:].rearrange("p e t -> p t e"), tot[:, 0:HT, :], ALU.subtract),
                  reads=["incl", "tot"], writes=["posf2"])
            kb.op("dve", lambda: nc.vector.tensor_tensor(posf[:].rearrange("p t e -> p (t e)"), pP[:, 0:HT * NE],
                                                         posf[:].rearrange("p t e -> p (t e)"), ALU.add),
                  reads=["ps:pP", "posf2"], writes=["posf2"])
            kb.op("dve", lambda: nc.vector.tensor_tensor(posf[:], posf[:], toth[:].unsqueeze(1).to_broadcast([128, HT, NE]), ALU.add),
                  reads=["posf2", "toth"], writes=["posf2"])
            kb.op("dve", lambda: nc.vector.tensor_scalar(pen[:], posf[:], CAP - 0.5, BIG, ALU.is_ge, ALU.mult), reads=["posf2"], writes=["pen"])
            kb.op("dve", lambda: nc.vector.tensor_tensor(posf[:], posf[:], pen[:], ALU.add), reads=["posf2", "pen"], writes=["posf2"])
            kb.op("dve", lambda: nc.vector.tensor_scalar(pen[:], sel[:, 0:HT, :], -BIG, BIG, ALU.mult, ALU.add), reads=["sel"], writes=["pen"])
            kb.op("dve", lambda: nc.vector.tensor_tensor(posf[:], posf[:], pen[:], ALU.add), reads=["posf2", "pen"], writes=["posf2"])
            kb.op("dve", lambda: nc.vector.tensor_tensor(posf[:], posf[:], eoff[:], ALU.add), reads=["posf2", "eoff"], writes=["posf2"])
            kb.op("dve", lambda: nc.vector.tensor_copy(posi[:], posf[:]), reads=["posf2"], writes=["posi"])
            kb.barrier()
        if upto <= 7:
            if dbg:
                dump("posi", posi[:], [128, HT, NE], I32, ["posi"])
            return finish()

        with ExitStack() as sX:
            wg = [sb(sX, f"wg{i}", [128, 8, D], BF16) for i in range(2)]
            wu = [sb(sX, f"wu{i}", [128, 8, D], BF16) for i in range(2)]
            wd = sb(sX, "wd", [128, 8, D], BF16)
            xsb = [sb(sX, f"xs{i}", [128, 4, ROW], BF16) for i in range(2)]
            xinT = sb(sX, "xinT", [128, 8, 512], BF16)
            hTe = sb(sX, "hTe", [128, 8, 512], BF16)
            sa = sb(sX, "sa", [128, 512], F32)
            ysb = sb(sX, "ysb", [128, D], F32)
            pT = ps(sX, "pTx", [128, 8, 128], BF16)
            pA = [ps(sX, f"pGa{i}", [128, 512], F32) for i in range(2)]
            pB_ = [ps(sX, f"pGb{i}", [128, 512], F32) for i in range(2)]
            pY = [ps(sX, f"pY{i}", [128, 512], F32) for i in range(2)]
            nexp = 8
            xidx = sb(sX, "xidx", [128, 32], I32)
            gt = sb(sX, "gt", [128, 8], F32)
            idf = sb(sX, "idf", [128, 4], F32)
            idt = sb(sX, "idt", [128, 4], I32)
            kb.dma("sp", xidx[:], A["xidx"], writes=["xidx"], sem="c8")
            reg_cap = nc.gpsimd.alloc_register("reg_cap")
            nc.gpsimd.reg_mov(reg_cap, NE * CAP - 1)
            reg_s = nc.gpsimd.alloc_register("reg_s")
            nc.gpsimd.reg_mov(reg_s, 2 * S - 1)

            def load_w(e):
                i = e % 2
                kb.dma("pool", wg[i][:], A["w_gate"][e].rearrange("(c p) n -> p c n", p=128), writes=[f"wg{i}"], sem=f"wg{i}")
                kb.dma("pool", wu[i][:], A["w_up"][e].rearrange("(c p) n -> p c n", p=128), writes=[f"wu{i}"], sem=f"wu{i}")

            load_w(0)
            kb.dma("pool", wd[:], A["w_down"][0].rearrange("(c p) n -> p c n", p=128), writes=["wd"], sem="wd")
            load_w(1)
            def scat(e, t):
                kb.dma_ins("pool", lambda t=t, e=e: nc.gpsimd.indirect_dma_start(
                    out=xin_sh, out_offset=bass.IndirectOffsetOnAxis(ap=posi[:, t, e:e + 1], axis=0),
                    in_=h3rows[:, t, :], in_offset=None, bounds_check=reg_cap, oob_is_err=False),
                    reads=["h3rows", "posi"], writes=[f"xin{e}_{t}"], sem=f"sc{e % 4}")

            first = [0, 1, 2, 3, 8, 9, 10, 11]
            later = [(e, t) for e in (4, 5, 6, 7, 12, 13, 14, 15) for t in range(HT)]
            for e in first:
                for t in range(HT):
                    scat(e, t)
            pair_barrier(2)
            def gath(e):
                xb = e % 2
                for s_ in range(4):
                    kb.dma_ins("pool", lambda s_=s_, e=e, xb=xb: nc.gpsimd.indirect_dma_start(
                        out=xsb[xb][:, s_, :], out_offset=None, in_=xin_sh,
                        in_offset=bass.IndirectOffsetOnAxis(ap=xidx[:, e * 4 + s_:e * 4 + s_ + 1], axis=0)),
                        reads=["xidx"], writes=[f"xs{xb}_{s_}"], sem=f"xs{xb}_{s_}")

            gath(0)
            for e in range(nexp):
                i = e % 2
                xs = xsb[e % 2]
                XSK = [f"xs{e % 2}_{q}" for q in range(4)]
                if e == 4:
                    pair_barrier(4)
                    gath(4)
                if e > 0:
                    kb.dma("pool", wd[:], A["w_down"][e].rearrange("(c p) n -> p c n", p=128), writes=["wd"], sem="wd")
                if 1 <= e and e + 1 < nexp:
                    load_w(e + 1)
                if e + 1 < nexp and e + 1 != 4:
                    gath(e + 1)
                if e < 4:
                    for (e2, t2) in later[e * 32:(e + 1) * 32]:
                        scat(e2, t2)
                affr = xs[:, :, 1024:1056].bitcast(F32)
                kb.op("dve", lambda affr=affr, e=e: nc.vector.tensor_tensor(gt[:, 0:4], affr[:, :, 8 + e], affr[:, :, e], ALU.subtract),
                      reads=XSK, writes=["gt"])
                kb.op("dve", lambda affr=affr, e=e: nc.vector.scalar_tensor_tensor(
                    gt[:, 4:8], gt[:, 0:4], jf[:, 0:1], affr[:, :, e], ALU.mult, ALU.add), reads=["gt", "jf"] + XSK, writes=["gt"])
                kb.op("dve", lambda xs=xs: nc.vector.tensor_copy(idf[:], xs[:, :, 1056:1058].bitcast(I32)[:, :, 0]), reads=XSK, writes=["idf"])
                kb.op("dve", lambda: nc.vector.tensor_scalar(idf[:], idf[:], joff[:, 0:1], None, ALU.add), reads=["idf", "joff"], writes=["idf"])
                kb.op("dve", lambda: nc.vector.tensor_copy(idt[:], idf[:]), reads=["idf"], writes=["idt"])
                for s_ in range(4):
                    for c in range(8):
                        kb.op("pe", lambda s_=s_, c=c, xs=xs: nc.tensor.transpose(pT[:, c, :], xs[:, s_, c * 128:(c + 1) * 128], ident_bf[:]),
                              reads=[XSK[s_], "ident_bf"], writes=["ps:pTx"], signal=(c == 7))
                    if s_ % 2 == 0:
                        kb.op("act", lambda s_=s_: nc.scalar.copy(xinT[:, :, s_ * 128:(s_ + 1) * 128], pT[:]), reads=["ps:pTx"], writes=["xinT"])
                    else:
                        kb.op("dve", lambda s_=s_: nc.vector.tensor_copy(xinT[:, :, s_ * 128:(s_ + 1) * 128], pT[:]), reads=["ps:pTx"], writes=["xinT"])
                for fc in range(8):
                    b = fc % 2
                    fs = slice(fc * 128, (fc + 1) * 128)
                    for c in range(8):
                        kb.op("pe", lambda b=b, c=c, fs=fs, i=i: nc.tensor.matmul(
                            pA[b][:], wg[i][:, c, fs], xinT[:, c, :], start=(c == 0), stop=(c == 7)),
                            reads=[f"wg{i}", "xinT"], writes=[f"ps:pGa{b}"], signal=(c == 7))
                    for c in range(8):
                        kb.op("pe", lambda b=b, c=c, fs=fs, i=i: nc.tensor.matmul(
                            pB_[b][:], wu[i][:, c, fs], xinT[:, c, :], start=(c == 0), stop=(c == 7)),
                            reads=[f"wu{i}", "xinT"], writes=[f"ps:pGb{b}"], signal=(c == 7))
                    kb.op("act", lambda b=b: nc.scalar.activation(sa[:], pA[b][:], AF.Silu), reads=[f"ps:pGa{b}"], writes=["sa"])
                    kb.op("dve", lambda b=b, fc=fc: nc.vector.tensor_tensor(hTe[:, fc, :], pB_[b][:], sa[:], ALU.mult),
                          reads=[f"ps:pGb{b}", "sa"], writes=["hTe"])
                for s_ in range(4):
                    ss_ = slice(s_ * 128, (s_ + 1) * 128)
                    gate_ap = gt[:, 4 + s_:5 + s_]
                    for half in range(2):
                        hs = slice(half * 512, (half + 1) * 512)
                        for fc in range(8):
                            kb.op("pe", lambda half=half, fc=fc, ss_=ss_, hs=hs: nc.tensor.matmul(
                                pY[half][:], hTe[:, fc, ss_], wd[:, fc, hs], start=(fc == 0), stop=(fc == 7)),
                                reads=["hTe", "wd"], writes=[f"ps:pY{half}"], signal=(fc == 7))
                        if half == 0:
                            kb.op("act", lambda hs=hs, gate_ap=gate_ap: nc.scalar.activation(
                                ysb[:, hs], pY[0][:], AF.Copy, scale=gate_ap), reads=["ps:pY0", "gt"], writes=["ysb"])
                        else:
                            kb.op("dve", lambda hs=hs, gate_ap=gate_ap: nc.vector.tensor_scalar(
                                ysb[:, hs], pY[1][:], gate_ap, None, ALU.mult), reads=["ps:pY1", "gt"], writes=["ysb"])
                    ids_ap = idt[:, s_:s_ + 1]
                    kb.dma_ins("pool", lambda ids_ap=ids_ap: nc.gpsimd.indirect_dma_start(
                        out=accp_sh, out_offset=bass.IndirectOffsetOnAxis(ap=ids_ap, axis=0),
                        in_=ysb[:], in_offset=None, bounds_check=reg_s, oob_is_err=True, compute_op=ALU.add),
                        reads=["ysb", "idt"] + [f"accz{q}" for q in range(HT)] + [f"accx{q}" for q in range(HT)],
                        writes=["accp"], sem="sa")
            pair_barrier(3)
        if upto <= 8:
            if dbg:
                dump("accp", accp_sh, [2 * S, D], F32, ["accp"])
            return finish()

        with ExitStack() as sN:
            gfin = sb(sN, "gfin", [128, D], F32)
            f0idx = sb(sN, "f0idx", [128, HT], I32)
            f1idx = sb(sN, "f1idx", [128, HT], I32)
            xf = [sb(sN, f"xf{i}", [128, D], F32) for i in range(4)]
            xg = [sb(sN, f"xg{i}", [128, D], F32) for i in range(4)]
            of = [sb(sN, f"of{i}", [128, D], F32) for i in range(4)]
            sqf = sb(sN, "sqf", [128, D], BF16)
            kb.dma("sp", gfin[:], A["g_final"], writes=["gfin"], sem="c6")
            kb.dma("sp", f0idx[:], A["f0idx"], writes=["f0idx"], sem="c6a")
            kb.dma("sp", f1idx[:], A["f1idx"], writes=["f1idx"], sem="c6b")
            for t in range(HT):
                i = t % 4
                kb.dma_ins("pool", lambda t=t, i=i: nc.gpsimd.indirect_dma_start(
                    out=xf[i][:], out_offset=None, in_=accp_sh,
                    in_offset=bass.IndirectOffsetOnAxis(ap=f0idx[:, t:t + 1], axis=0)),
                    reads=["f0idx", "accp"], writes=[f"xf{i}"], sem=f"xf{i}")
                kb.dma_ins("pool", lambda t=t, i=i: nc.gpsimd.indirect_dma_start(
                    out=xg[i][:], out_offset=None, in_=accp_sh,
                    in_offset=bass.IndirectOffsetOnAxis(ap=f1idx[:, t:t + 1], axis=0)),
                    reads=["f1idx", "accp"], writes=[f"xg{i}"], sem=f"xg{i}")
                kb.op("dve", lambda i=i: nc.vector.tensor_tensor(xf[i][:], xf[i][:], xg[i][:], ALU.add),
                      reads=[f"xf{i}", f"xg{i}"], writes=[f"xf{i}"])
                kb.op("act", lambda i=i, t=t: nc.scalar.activation(sqf[:], xf[i][:], AF.Square, accum_out=ssall[:, 5, t:t + 1]),
                      reads=[f"xf{i}"], writes=["sqf", f"ss5_{t}"])
                rstd_of(5, t, D)
                kb.op("dve", lambda i=i, t=t: nc.vector.scalar_tensor_tensor(
                    of[i][:], xf[i][:], rsall[:, 5, t:t + 1], gfin[:], ALU.mult, ALU.mult),
                    reads=[f"xf{i}", f"rs5_{t}", "gfin"], writes=[f"of{i}"])
                kb.dma("sp", out_d[t * 128:(t + 1) * 128, :], of[i][:], reads=[f"of{i}"], writes=["out"], sem=f"of{i}")
            kb.barrier()
        return finish()


_CACHE = {}


def kernel(**inputs):
    inp = {k: np.asarray(v) for k, v in inputs.items()}
    if "nc" not in _CACHE:
        _CACHE["nc"] = build()[0]
    nc = _CACHE["nc"]
    consts = host_consts()
    in_maps = []
    for core in range(8):
        m = host_layout(inp, core // 2, core % 2)
        m.update(consts)
        in_maps.append(m)
    res = run_bass_kernel_spmd(nc, in_maps, core_ids=list(range(8)))
    out = np.empty((4, S, D), np.float32)
    for core in range(8):
        out[core // 2, (core % 2) * 2048:(core % 2 + 1) * 2048] = np.asarray(res.results[core]["out"])
    return out
```

```python
import math, os
import numpy as np
import ml_dtypes
from contextlib import ExitStack
import concourse.bass as bass
import concourse.mybir as mybir
from concourse.bass_utils import run_bass_kernel_spmd

F32 = mybir.dt.float32
BF16 = mybir.dt.bfloat16
I32 = mybir.dt.int32
AF = mybir.ActivationFunctionType
ALU = mybir.AluOpType
AX = mybir.AxisListType

S = 4096
D = 1024
T = 32
EPS = 1e-6
NE = 16
CAP = 512
ROW = 1064
BIG = 100000.0


class KB:
    SEM_ROLL = 24000

    def __init__(self, nc, stack):
        self.nc = nc
        self.stack = stack
        self.engs = {"pe": nc.tensor, "act": nc.scalar, "dve": nc.vector,
                     "pool": nc.gpsimd, "sp": nc.sync}
        self.esem = {}
        self.nsem = 0
        for e in self.engs:
            self._new_esem(e)
        self.dsem = {}
        self.waited = {}
        self.lastw = {}
        self.readers = {}

    def _mk(self, name):
        self.nsem += 1
        return self.stack.enter_context(self.nc.semaphore(name))

    def _new_esem(self, e):
        gen = 0 if e not in self.esem else self.esem[e][2] + 1
        self.esem[e] = [self._mk(f"s_{e}_{gen}"), 0, gen]

    def _deps(self, reads, writes):
        deps = []
        for k in reads:
            t = self.lastw.get(k)
            if t is not None:
                deps.append(t)
        for k in writes:
            t = self.lastw.get(k)
            if t is not None:
                deps.append(t)
            deps.extend(self.readers.get(k, ()))
        return deps

    def _wait(self, e, deps, skip_self=False):
        best = {}
        for (h, v, pe) in deps:
            if skip_self and pe == e:
                continue
            if h.name not in best or best[h.name][1] < v:
                best[h.name] = (h, v)
        for nm, (h, v) in best.items():
            if self.waited.get((e, nm), 0) >= v:
                continue
            self.engs[e].wait_ge(h, v)
            self.waited[(e, nm)] = v

    def _record(self, tok, reads, writes):
        for k in writes:
            self.lastw[k] = tok
            self.readers[k] = []
        for k in reads:
            self.readers.setdefault(k, []).append(tok)

    def op(self, e, fn, reads=(), writes=(), signal=True, extra=()):
        writes = list(writes) + [k for k in reads if k.startswith("ps:") and k not in writes]
        deps = self._deps(reads, writes) + list(extra)
        self._wait(e, deps, skip_self=(e == "pe"))
        ins = fn()
        s = self.esem[e]
        if signal:
            s[1] += 1
            ins.then_inc(s[0], 1)
            tok = (s[0], s[1], e)
            if s[1] >= self.SEM_ROLL:
                self._new_esem(e)
        else:
            tok = (s[0], s[1] + 1, e)
        self._record(tok, reads, writes)
        return tok

    def _dsem(self, sem):
        if sem not in self.dsem:
            self.dsem[sem] = [self._mk(f"d_{sem}"), 0]
        s = self.dsem[sem]
        if s[1] >= self.SEM_ROLL:
            self.dsem[sem] = s = [self._mk(f"d_{sem}_{self.nsem}"), 0]
        return s

    def dma(self, q, out, in_, reads=(), writes=(), sem="d0", extra=(), **kw):
        deps = self._deps(reads, writes) + list(extra)
        self._wait(q, deps)
        s = self._dsem(sem)
        ins = self.engs[q].dma_start(out=out, in_=in_, **kw)
        s[1] += 16
        ins.then_inc(s[0], 16)
        tok = (s[0], s[1], "dma")
        self._record(tok, reads, writes)
        return tok

    def dma_ins(self, q, fn, reads=(), writes=(), sem="d0", extra=()):
        deps = self._deps(reads, writes) + list(extra)
        self._wait(q, deps)
        s = self._dsem(sem)
        ins = fn()
        s[1] += 16
        ins.then_inc(s[0], 16)
        tok = (s[0], s[1], "dma")
        self._record(tok, reads, writes)
        return tok

    def join(self, keys, sem):
        s = self.dsem[sem]
        tok = (s[0], s[1], "dma")
        for k in keys:
            self.lastw[k] = tok

    def all_tokens(self):
        toks = [(s[0], s[1], e) for e, s in self.esem.items() if s[1] > 0]
        toks += [(s[0], s[1], "dma") for s in self.dsem.values() if s[1] > 0]
        return toks

    def barrier(self, engines=("pe", "act", "dve", "pool", "sp")):
        toks = self.all_tokens()
        for e in engines:
            self._wait(e, toks)


def _bf(a):
    return np.ascontiguousarray(a).astype(ml_dtypes.bfloat16)


def host_consts():
    c = {}
    c["ident_bf"] = _bf(np.eye(128))
    c["ident_f"] = np.eye(128, dtype=np.float32)
    c["ones_bf"] = _bf(np.ones((128, 128)))
    c["ones_f"] = np.ones((128, 128), np.float32)
    c["ustrict"] = _bf(np.triu(np.ones((128, 128)), 1))
    i64 = np.arange(64)
    a = 2 * np.pi * np.outer(i64, i64) / 64.0
    f1 = np.concatenate([np.cos(a), -np.sin(a)], axis=1)
    c["f1"] = _bf(np.concatenate([f1, f1], axis=0))
    s2 = i64[:, None, None]
    k1 = i64[None, :, None]
    k2 = i64[None, None, :]
    ang = 2 * np.pi * s2 * (k1 + 64 * k2) / 4096.0
    wr, wi = np.cos(ang), -np.sin(ang)
    ta = np.concatenate([wr, wi], axis=2)
    tb = np.concatenate([-wi, wr], axis=2)
    c["tabA"] = _bf(np.concatenate([ta, ta], axis=0))
    c["tabB"] = _bf(np.concatenate([tb, tb], axis=0))
    lb = np.zeros((4, 128, 128), np.float64)
    for c2 in range(2):
        for ri in range(2):
            for j in range(2):
                for i in range(32):
                    ch = 2 * i + c2
                    m = np.arange(64)
                    coef = (np.cos if ri == 0 else np.sin)(2 * np.pi * ch * m / 64.0) / 512.0
                    for oc in range(2):
                        lb[c2 * 2 + ri, j * 64:(j + 1) * 64, oc * 64 + j * 32 + i] = coef
    c["lbig"] = _bf(lb.transpose(1, 0, 2))
    fr = 1.0 / (10000.0 ** (np.arange(0, 32, 2, dtype=np.float32) / 32.0))
    c["freq"] = np.ascontiguousarray(np.broadcast_to(fr.astype(np.float32)[None, :], (128, 16)))
    c["eoff"] = np.ascontiguousarray(np.broadcast_to((512.0 * np.arange(16, dtype=np.float32))[None, None, :], (128, 16, 16)))
    return c


def host_layout(inp, b, j):
    m = {}
    m["x"] = np.ascontiguousarray(inp["x"][b])
    m["xh"] = np.ascontiguousarray(inp["x"][b][2048 * j:2048 * (j + 1)])
    m["mem"] = np.ascontiguousarray(inp["mem"][b])
    m["posT"] = np.ascontiguousarray(inp["positions"][b].reshape(32, 128).T)
    w_in = inp["w_in"][0]
    kr = w_in[:, 1152:1184]
    kr_sw = np.concatenate([kr[:, 16:32], kr[:, 0:16]], axis=1)
    m["w_in"] = np.ascontiguousarray(np.concatenate(
        [w_in[:, 256 * j:256 * (j + 1)], w_in[:, 512:1184], kr_sw], axis=1))
    wq = inp["w_q_up"][0].reshape(384, 8, 96)
    hs = wq[:, 4 * j:4 * j + 4, :]
    rope = hs[:, :, 64:96]
    sw = np.concatenate([rope[:, :, 16:32], rope[:, :, 0:16]], axis=2).reshape(384, 128)
    m["w_q"] = np.ascontiguousarray(np.concatenate([hs.reshape(384, 384), sw], axis=1))
    wkv = inp["w_kv_up"][0].reshape(256, 8, 128)[:, 4 * j:4 * j + 4, :]
    m["w_kv"] = np.ascontiguousarray(np.concatenate(
        [wkv[:, :, 0:64].reshape(256, 256), wkv[:, :, 64:128].reshape(256, 256)], axis=1))
    wf = inp["w_fourier"][0]
    r = np.zeros((2, 128, 128), np.float32)
    for oc in range(2):
        for jj in range(2):
            r[oc, jj * 64:(jj + 1) * 64, jj * 64:(jj + 1) * 64] = wf[4 * j + 2 * oc + jj]
    m["wf_r"] = np.ascontiguousarray(r.transpose(1, 0, 2))
    m["w_out"] = np.ascontiguousarray(inp["w_out"][0])
    m["w_mem_q"] = np.ascontiguousarray(inp["w_mem_q"][0])
    m["w_mem_kv"] = np.ascontiguousarray(inp["w_mem_kv"][0])
    m["w_mem_o"] = np.ascontiguousarray(inp["w_mem_o"][0])
    m["w_router"] = np.ascontiguousarray(inp["w_router"][0])
    m["w_gate"] = np.ascontiguousarray(inp["w_exp_gate"][0][8 * j:8 * j + 8])
    m["w_up"] = np.ascontiguousarray(inp["w_exp_up"][0][8 * j:8 * j + 8])
    m["w_down"] = np.ascontiguousarray(inp["w_exp_down"][0][8 * j:8 * j + 8])
    rep = lambda v: np.ascontiguousarray(np.broadcast_to(v[None, :], (128, v.shape[0])))
    m["g_mix"] = rep(inp["g_mix"][0])
    m["g_mem_q"] = rep(inp["g_mem_q"][0])
    m["g_mem_kv"] = rep(inp["g_mem_kv"][0])
    m["g_ffn"] = rep(inp["g_ffn"][0])
    m["g_final"] = rep(inp["g_final"])
    m["g_q_lat"] = np.ascontiguousarray(inp["g_q_lat"][0].reshape(3, 128).T)
    m["g_kv_lat"] = np.ascontiguousarray(inp["g_kv_lat"][0].reshape(2, 128).T)
    p = np.arange(128)[:, None]
    i32 = lambda a: np.ascontiguousarray(a).astype(np.int32)
    cks = np.array([2 * j, 2 * j + 1, 4 + 2 * j, 4 + 2 * j + 1])[None, :]
    m["yidx"] = i32((cks * 128 + p) * 8)
    m["gidx"] = i32((np.arange(8)[None, :] * 128 + p) * 8 + 4 * j)
    m["hidx"] = i32(j * 128 + p)
    m["aidx"] = i32(np.concatenate([j * 128 + p, (1 - j) * 128 + p], axis=1))
    el = np.arange(8)[None, :, None]
    st_ = np.arange(4)[None, None, :]
    m["xidx"] = i32(((8 * j + el) * 512 + st_ * 128 + p[:, :, None]).reshape(128, 32))
    tl = np.arange(16)[None, :]
    gid = 2048 * j + tl * 128 + p
    oid = 2048 * (1 - j) + tl * 128 + p
    m["tokid"] = i32(gid)
    m["x2idx"] = i32(4096 * j + gid)
    m["zidx"] = i32(4096 * j + oid)
    m["f0idx"] = i32(gid)
    m["f1idx"] = i32(4096 + gid)
    m["jf"] = np.full((128, 1), float(j), np.float32)
    m["joff"] = np.full((128, 1), 4096.0 * j, np.float32)
    return m


IN_SPECS = [
    ("x", [S, D], F32), ("xh", [S // 2, D], F32), ("mem", [256, D], F32), ("posT", [128, 32], I32),
    ("w_in", [D, 960], F32), ("w_q", [384, 512], F32), ("w_kv", [256, 512], F32),
    ("wf_r", [128, 2, 128], F32), ("w_out", [D, D], F32), ("w_mem_q", [D, D], F32),
    ("w_mem_kv", [D, 2048], F32), ("w_mem_o", [D, D], F32), ("w_router", [D, NE], F32),
    ("w_gate", [8, D, D], F32), ("w_up", [8, D, D], F32), ("w_down", [8, D, D], F32),
    ("g_mix", [128, D], F32), ("g_mem_q", [128, D], F32), ("g_mem_kv", [128, D], F32),
    ("g_ffn", [128, D], F32), ("g_final", [128, D], F32), ("g_q_lat", [128, 3], F32),
    ("g_kv_lat", [128, 2], F32),
    ("ident_bf", [128, 128], BF16), ("ident_f", [128, 128], F32), ("ones_bf", [128, 128], BF16),
    ("ones_f", [128, 128], F32), ("ustrict", [128, 128], BF16), ("f1", [128, 128], BF16),
    ("tabA", [128, 64, 128], BF16), ("tabB", [128, 64, 128], BF16), ("lbig", [128, 4, 128], BF16),
    ("freq", [128, 16], F32), ("eoff", [128, 16, 16], F32),
    ("yidx", [128, 4], I32), ("gidx", [128, 8], I32), ("hidx", [128, 1], I32), ("aidx", [128, 2], I32),
    ("xidx", [128, 32], I32), ("tokid", [128, 16], I32), ("x2idx", [128, 16], I32), ("zidx", [128, 16], I32),
    ("f0idx", [128, 16], I32), ("f1idx", [128, 16], I32), ("jf", [128, 1], F32), ("joff", [128, 1], F32),
]
HT = 16


def build(upto=99, dbg=None):
    nc = bass.Bass("TRN2", target_bir_lowering=False)
    A = {}
    for name, shape, dt in IN_SPECS:
        A[name] = nc.dram_tensor(name, shape, dt, kind="ExternalInput").ap()
    out_d = nc.dram_tensor("out", [S // 2, D], F32, kind="ExternalOutput").ap()
    uT_d = nc.dram_tensor("uT_d", [6, 128, S], BF16).ap()
    yT_sh = nc.dram_tensor("yT_sh", [8 * 128 * 8, 512], BF16, addr_space="Shared").ap()
    affs_sh = nc.dram_tensor("affs_sh", [256, 256], F32, addr_space="Shared").ap()
    xin_sh = nc.dram_tensor("xin_sh", [NE * CAP, ROW], BF16, addr_space="Shared").ap()
    accp_sh = nc.dram_tensor("accp_sh", [2 * S, D], F32, addr_space="Shared").ap()
    bar_in = [nc.dram_tensor(f"bar_in{i}", [16, 16], F32).ap() for i in range(5)]
    bar_out = [nc.dram_tensor(f"bar_out{i}", [32, 16], F32).ap() for i in range(5)]
    dbg_out = {}

    with ExitStack() as st:
        kb = KB(nc, st)
        E = kb.engs

        def sb(stack, n, s, d):
            return stack.enter_context(nc.sbuf_tensor("sb_" + n, s, d))

        def ps(stack, n, s, d):
            return stack.enter_context(nc.psum_tensor("ps_" + n, s, d))

        def dump(name, src_ap, shape, dt, reads):
            o = nc.dram_tensor("dbg_" + name, shape, dt, kind="ExternalOutput").ap()
            dbg_out[name] = o
            kb.barrier()
            kb.dma("sp", o, src_ap, reads=reads, writes=["dbg_" + name], sem="dbg")

        def finish():
            kb.barrier()
            return nc, dbg_out

        def pair_barrier(i):
            kb.barrier()
            kb.dma("sp", bar_in[i], ones_f[0:16, 0:16], reads=["ones_f"], writes=[f"bar_in{i}"], sem="bar")
            kb.op("pool", lambda: nc.gpsimd.collective_compute(
                "AllGather", ALU.bypass, replica_groups=[[0, 1], [2, 3], [4, 5], [6, 7]],
                ins=[bar_in[i]], outs=[bar_out[i]]), reads=[f"bar_in{i}"], writes=[f"bar_out{i}"])
            kb.barrier()

        ident_bf = sb(st, "ident_bf", [128, 128], BF16)
        ident_f = sb(st, "ident_f", [128, 128], F32)
        ones_bf = sb(st, "ones_bf", [128, 128], BF16)
        ones_f = sb(st, "ones_f", [128, 128], F32)
        ustrict = sb(st, "ustrict", [128, 128], BF16)
        ssall = sb(st, "ssall", [128, 6, T], F32)
        rsall = sb(st, "rsall", [128, 6, T], F32)
        affl = sb(st, "affl", [128, HT, NE], F32)
        affs = sb(st, "affs", [128, T, NE], F32)
        posi = sb(st, "posi", [128, HT, NE], I32)
        jf = sb(st, "jf", [128, 1], F32)
        joff = sb(st, "joff", [128, 1], F32)
        yidx = sb(st, "yidx", [128, 4], I32)
        kb.dma("sp", jf[:], A["jf"], writes=["jf"], sem="c0")
        kb.dma("sp", joff[:], A["joff"], writes=["joff"], sem="c0")
        kb.dma("sp", yidx[:], A["yidx"], writes=["yidx"], sem="c0")
        kb.join(["ident_bf", "ident_f", "ones_bf", "ones_f", "ustrict", "jf", "joff", "yidx"], "c0")
        rq = sb(st, "rq", [128, T], F32)
        rkv = sb(st, "rkv", [128, T], F32)
        for nm, t in (("ident_bf", ident_bf), ("ident_f", ident_f), ("ones_bf", ones_bf),
                      ("ones_f", ones_f), ("ustrict", ustrict)):
            kb.dma("sp", t[:], A[nm], writes=[nm], sem="c0")
        kb.op("pool", lambda: nc.gpsimd.memset(ssall[:], 0.0), writes=["ssall"])
        if upto <= -3:
            return finish()

        big1 = sb(st, "big1", [128, 34816], BF16)
        sAD = ExitStack()
        cos2 = sb(sAD, "cos2", [128, T, 32], F32)
        sin2s = sb(sAD, "sin2s", [128, T, 32], F32)
        with ExitStack() as s0:
            posT = sb(s0, "posT", [128, T], I32)
            posf = sb(s0, "posf", [128, T], F32)
            freq = sb(s0, "freq", [128, 16], F32)
            ang = sb(s0, "ang", [128, T, 16], F32)
            kk = sb(s0, "kk", [128, T, 16], I32)
            kf = sb(s0, "kf", [128, T, 16], F32)
            rr = sb(s0, "rr", [128, T, 16], F32)
            mk = sb(s0, "mk", [128, T, 16], F32)
            sn = sb(s0, "sn", [128, T, 16], F32)
            kb.dma("sp", posT[:], A["posT"], writes=["posT"], sem="c0p")
            kb.dma("sp", freq[:], A["freq"], writes=["freq"], sem="c0f")
            kb.op("dve", lambda: nc.vector.tensor_copy(posf[:], posT[:]), reads=["posT"], writes=["posf"])
            kb.op("dve", lambda: nc.vector.tensor_tensor(
                ang[:], posf[:].unsqueeze(2).to_broadcast([128, T, 16]),
                freq[:].unsqueeze(1).to_broadcast([128, T, 16]), ALU.mult),
                reads=["posf", "freq"], writes=["ang"])
            if upto <= -2:
                dump("ang", ang[:], [128, T, 16], F32, ["ang"])
                return finish()

            def sin_of(shift, outs):
                TWO_PI = 2.0 * math.pi
                kb.op("dve", lambda: nc.vector.tensor_scalar(kf[:], ang[:], shift, 1.0 / TWO_PI, ALU.add, ALU.mult),
                      reads=["ang"], writes=["kf"])
                kb.op("dve", lambda: nc.vector.tensor_copy(kk[:], kf[:]), reads=["kf"], writes=["kk"])
                kb.op("dve", lambda: nc.vector.tensor_copy(kf[:], kk[:]), reads=["kk"], writes=["kf"])
                kb.op("dve", lambda: nc.vector.scalar_tensor_tensor(rr[:], kf[:], -TWO_PI, ang[:], ALU.mult, ALU.add),
                      reads=["kf", "ang"], writes=["rr"])
                if shift != 0.0:
                    kb.op("dve", lambda: nc.vector.tensor_scalar(rr[:], rr[:], shift, None, ALU.add),
                          reads=["rr"], writes=["rr"])
                for _ in range(2):
                    kb.op("dve", lambda: nc.vector.tensor_scalar(mk[:], rr[:], math.pi, -TWO_PI, ALU.is_gt, ALU.mult),
                          reads=["rr"], writes=["mk"])
                    kb.op("dve", lambda: nc.vector.tensor_tensor(rr[:], rr[:], mk[:], ALU.add),
                          reads=["rr", "mk"], writes=["rr"])
                    kb.op("dve", lambda: nc.vector.tensor_scalar(mk[:], rr[:], -math.pi, TWO_PI, ALU.is_lt, ALU.mult),
                          reads=["rr"], writes=["mk"])
                    kb.op("dve", lambda: nc.vector.tensor_tensor(rr[:], rr[:], mk[:], ALU.add),
                          reads=["rr", "mk"], writes=["rr"])
                kb.op("dve", lambda: nc.vector.tensor_scalar(rr[:], rr[:], math.pi, -math.pi, ALU.min, ALU.max),
                      reads=["rr"], writes=["rr"])
                if upto == -1:
                    return
                kb.op("act", lambda: nc.scalar.activation(sn[:], rr[:], AF.Sin), reads=["rr"], writes=["sn"])
                for dst, sign in outs:
                    kb.op("dve", lambda dst=dst, sign=sign: nc.vector.tensor_scalar(dst, sn[:], sign, None, ALU.mult),
                          reads=["sn"], writes=["rope_tab"])

            sin_of(0.0, [(sin2s[:, :, 0:16], -1.0), (sin2s[:, :, 16:32], 1.0)])
            if upto == -1:
                dump("rr", rr[:], [128, T, 16], F32, ["rr"])
                dump("ang", ang[:], [128, T, 16], F32, ["ang"])
                return finish()
            sin_of(0.5 * math.pi, [(cos2[:, :, 0:16], 1.0), (cos2[:, :, 16:32], 1.0)])
            kb.barrier()
        if upto <= 0:
            if dbg:
                dump("cos2", cos2[:], [128, T, 32], F32, ["rope_tab"])
                dump("sin2s", sin2s[:], [128, T, 32], F32, ["rope_tab"])
            return finish()

        hT = big1[:, 0:8 * S].rearrange("p (c s) -> p c s", c=8)
        with ExitStack() as sU:
            U = sb(sU, "U", [64, 256, 64], BF16)
            with ExitStack() as s1:
                Wb = sb(s1, "Wb", [128, 8, 960], BF16)
                gmix = sb(s1, "gmix", [128, D], F32)
                xt = [sb(s1, f"xt{i}", [128, D], F32) for i in range(2)]
                hb = [sb(s1, f"hb{i}", [128, D], BF16) for i in range(2)]
                sqj = sb(s1, "sqj", [128, D], BF16)
                sqt = [sb(s1, f"sqt{i}", [128, 512], BF16) for i in range(2)]
                ust = [sb(s1, f"ust{i}", [128, 512], BF16) for i in range(2)]
                tmpc = sb(s1, "tmpc", [128, 2 * T], F32)
                pT = [ps(s1, f"ps:pT{i}", [128, 8, 128], BF16) for i in range(2)]
                pB = [ps(s1, f"ps:pB{i}", [128, 512], F32) for i in range(2)]
                pss = ps(s1, "ps:pss", [128, 512], F32)
                for c in range(8):
                    kb.dma("pool", Wb[:, c, :], A["w_in"][c * 128:(c + 1) * 128, :], writes=[f"Wb_{c}"], sem="wb")
                kb.join(["Wb"], "wb")
                kb.dma("sp", gmix[:], A["g_mix"], writes=["gmix"], sem="c0g")
                kb.op("dve", lambda: nc.vector.memset(pss[:], 0.0), writes=["ps:pss"])
                def a_front(t):
                    i = t % 2
                    kb.dma("sp", xt[i][:], A["x"][t * 128:(t + 1) * 128, :], writes=[f"xt{i}"], sem=f"x{i}")
                    kb.op("act", lambda i=i, t=t: nc.scalar.activation(
                        sqj[:], xt[i][:], AF.Square, accum_out=ssall[:, 0, t:t + 1]),
                        reads=[f"xt{i}"], writes=["sqj", f"ssA{t}"])
                    kb.op("act", lambda t=t: nc.scalar.activation(
                        rsall[:, 0, t:t + 1], ssall[:, 0, t:t + 1], AF.Sqrt, bias=EPS, scale=1.0 / D),
                        reads=[f"ssA{t}"], writes=[f"rsA{t}"])
                    kb.op("dve", lambda t=t: nc.vector.reciprocal(rsall[:, 0, t:t + 1], rsall[:, 0, t:t + 1]),
                          reads=[f"rsA{t}"], writes=[f"rsA{t}"])
                    kb.op("dve", lambda i=i, t=t: nc.vector.scalar_tensor_tensor(
                        hb[i][:], xt[i][:], rsall[:, 0, t:t + 1], gmix[:], ALU.mult, ALU.mult),
                        reads=[f"xt{i}", f"rsA{t}", "gmix"], writes=[f"hb{i}"])

                def a_back(t):
                    i = t % 2
                    for c in range(8):
                        kb.op("pe", lambda i=i, c=c: nc.tensor.transpose(
                            pT[i][:, c, :], hb[i][:, c * 128:(c + 1) * 128], ident_bf[:]),
                            reads=[f"hb{i}", "ident_bf"], writes=[f"ps:pT{i}"], signal=(c == 7))
                    if t % 2 == 0:
                        kb.op("act", lambda i=i, t=t: nc.scalar.copy(hT[:, :, t * 128:(t + 1) * 128], pT[i][:]),
                              reads=[f"ps:pT{i}"], writes=[f"hT{t // 4}"])
                    else:
                        kb.op("dve", lambda i=i, t=t: nc.vector.tensor_copy(hT[:, :, t * 128:(t + 1) * 128], pT[i][:]),
                              reads=[f"ps:pT{i}"], writes=[f"hT{t // 4}"])

                a_front(0)
                for t in range(T):
                    if t + 1 < T:
                        a_front(t + 1)
                    a_back(t)
                if upto <= 1:
                    if dbg:
                        dump("hT", big1[:, 0:8 * S], [128, 8 * S], BF16, [f"hT{n}" for n in range(8)])
                    return finish()
                for s2 in range(64 if 'bf' not in os.environ.get('KSKIP', '') else 0):
                    i = s2 % 2
                    for c in range(8):
                        kb.op("pe", lambda i=i, c=c, s2=s2: nc.tensor.matmul(
                            pB[i][0:64, 0:256], hT[:, c, s2:S:64], Wb[:, c, 0:256],
                            start=(c == 0), stop=(c == 7)),
                            reads=[f"hT{n}" for n in range(8)] + ["Wb"], writes=[f"ps:pB{i}"],
                            signal=(c == 7))
                    if s2 % 2 == 0:
                        kb.op("act", lambda i=i, s2=s2: nc.scalar.copy(U[:, :, s2], pB[i][0:64, 0:256]),
                              reads=[f"ps:pB{i}"], writes=["U"])
                    else:
                        kb.op("dve", lambda i=i, s2=s2: nc.vector.tensor_copy(U[:, :, s2], pB[i][0:64, 0:256]),
                              reads=[f"ps:pB{i}"], writes=["U"])
                cnt = 0
                deferred = []
                for n in range(8):
                    for m in range(6):
                        M = 128 if m < 5 else 64
                        i = cnt % 2
                        cnt += 1
                        for c in range(8):
                            kb.op("pe", lambda i=i, m=m, M=M, c=c, n=n: nc.tensor.matmul(
                                pB[i][0:M, :], Wb[:, c, 256 + m * 128:256 + m * 128 + M],
                                hT[:, c, n * 512:(n + 1) * 512], start=(c == 0), stop=(c == 7)),
                                reads=[f"hT{n}", "Wb"], writes=[f"ps:pB{i}"], signal=(c == 7))
                        while deferred:
                            deferred.pop(0)()
                        tkc = kb.op("dve", lambda i=i, M=M: nc.vector.tensor_copy(ust[i][0:M, :], pB[i][0:M, :]),
                              reads=[f"ps:pB{i}"], writes=[f"ust{i}"])
                        if 'ud' not in os.environ.get('KSKIP', ''):
                            kb.dma("sp", uT_d[m, 0:M, n * 512:(n + 1) * 512], ust[i][0:M, :],
                                   reads=[f"ust{i}"], writes=["uT_d"], sem=f"us{i}")
                        if m < 5 and 'sq' not in os.environ.get('KSKIP', ''):
                            kb.op("act", lambda i=i: nc.scalar.activation(sqt[i][:], pB[i][:], AF.Square),
                                  reads=[f"ps:pB{i}"], writes=[f"sqt{i}"], extra=[tkc])
                            col0 = 0 if m < 3 else T

                            def ssq_mm(i=i, col0=col0, n=n):
                                for j in range(4):
                                    tcol = col0 + n * 4 + j
                                    kb.op("pe", lambda i=i, j=j, tcol=tcol: nc.tensor.matmul(
                                        pss[:, tcol:tcol + 1], sqt[i][:, j * 128:(j + 1) * 128], ones_bf[:, 0:1],
                                        start=False, stop=True, skip_group_check=True),
                                        reads=[f"sqt{i}", "ones_bf", "ps:pss"], writes=["ps:pss"], signal=(j == 3))
                            deferred.append(ssq_mm)
                while deferred:
                    deferred.pop(0)()
                kb.op("act", lambda: nc.scalar.activation(tmpc[:, 0:T], pss[:, 0:T], AF.Sqrt, bias=EPS, scale=1.0 / 384),
                      reads=["ps:pss"], writes=["tmpc"])
                kb.op("act", lambda: nc.scalar.activation(tmpc[:, T:2 * T], pss[:, T:2 * T], AF.Sqrt, bias=EPS, scale=1.0 / 256),
                      reads=["ps:pss"], writes=["tmpc"])
                kb.op("dve", lambda: nc.vector.reciprocal(rq[:], tmpc[:, 0:T]), reads=["tmpc"], writes=["rq"])
                kb.op("dve", lambda: nc.vector.reciprocal(rkv[:], tmpc[:, T:2 * T]), reads=["tmpc"], writes=["rkv"])
                kb.barrier()
            if upto <= 2:
                if dbg:
                    dump("U", U[:], [64, 256, 64], BF16, ["U"])
                    dump("uT", uT_d, [6, 128, S], BF16, ["uT_d"])
                    dump("rq", rq[:], [128, T], F32, ["rq"])
                    dump("rkv", rkv[:], [128, T], F32, ["rkv"])
                return finish()

            with ExitStack() as s2_:
                f1 = sb(s2_, "f1", [128, 128], BF16)
                tabA = sb(s2_, "tabA", [128, 64, 128], BF16)
                tabB = sb(s2_, "tabB", [128, 64, 128], BF16)
                lbig = sb(s2_, "lbig", [128, 4, 128], BF16)
                wfr = sb(s2_, "wfr", [128, 2, 128], BF16)
                Mb = sb(s2_, "Mb", [128, 8, 128], BF16)
                yst = [sb(s2_, f"yst{i}", [128, 512], BF16) for i in range(2)]
                pF = [ps(s2_, f"pF{i}", [128, 512], F32) for i in range(2)]
                pG = [ps(s2_, f"pG{i}", [128, 512], F32) for i in range(2)]
                pH = [ps(s2_, f"pH{i}", [128, 512], F32) for i in range(2)]
                Tt = big1[:, 0:16384].rearrange("p (cp k) -> p cp k", k=128)
                X = big1[:, 16384:32768].rearrange("p (c r k) -> p c r k", c=2, r=2)
                kb.dma("sp", f1[:], A["f1"], writes=["f1"], sem="c1")
                kb.dma("sp", tabA[:], A["tabA"], writes=["tabA"], sem="c1")
                kb.dma("sp", tabB[:], A["tabB"], writes=["tabB"], sem="c1")
                kb.dma("sp", lbig[:], A["lbig"], writes=["lbig"], sem="c1")
                kb.join(["f1", "tabA", "tabB", "lbig"], "c1")
                kb.dma("pool", wfr[:], A["wf_r"], writes=["wfr"], sem="c2")
                for og in range(2):
                    b = og % 2
                    for q4 in range(4):
                        kb.op("pe", lambda b=b, q4=q4, og=og: nc.tensor.matmul(
                            pF[b][:, q4 * 128:(q4 + 1) * 128], lbig[:, q4, :], wfr[:, og, :], start=True, stop=True),
                            reads=["lbig", "wfr"], writes=[f"ps:pF{b}"], signal=(q4 == 3))
                    ocl = og % 2
                    kb.op("dve", lambda b=b, og=og, ocl=ocl: nc.vector.tensor_copy(
                        Mb[64 * ocl:64 * ocl + 64, og * 4:(og + 1) * 4, :],
                        pF[b][64 * ocl:64 * ocl + 64, :].rearrange("p (q k) -> p q k", q=4)),
                        reads=[f"ps:pF{b}"], writes=["Mb"])
                ev = 0
                for fh in range(1):
                    for cp in range(128):
                        b = (cp // 4) % 2
                        c0 = fh * 256 + 2 * cp
                        kb.op("pe", lambda b=b, cp=cp, c0=c0: nc.tensor.matmul(
                            pF[b][:, (cp % 4) * 128:(cp % 4 + 1) * 128],
                            U[0:64, c0:c0 + 2, :].rearrange("p c s -> p (c s)"), f1[0:64, :], start=True, stop=True),
                            reads=["U", "f1"], writes=[f"ps:pF{b}"], signal=(cp % 4 == 3))
                        if cp % 4 == 3:
                            eng = "act" if ev % 2 == 0 else "dve"
                            ev += 1
                            dst = Tt[:, cp - 3:cp + 1, :]
                            src = pF[b][:].rearrange("p (q k) -> p q k", q=4)
                            if eng == "act":
                                kb.op("act", lambda dst=dst, src=src: nc.scalar.copy(dst, src), reads=[f"ps:pF{b}"], writes=["Tt"])
                            else:
                                kb.op("dve", lambda dst=dst, src=src: nc.vector.tensor_copy(dst, src), reads=[f"ps:pF{b}"], writes=["Tt"])
                    for c2 in range(2):
                        r0 = c2 * 64
                        for k1 in range(64):
                            b = (k1 // 4) % 2
                            o_ = pG[b][:, (k1 % 4) * 128:(k1 % 4 + 1) * 128]
                            kb.op("pe", lambda o_=o_, r0=r0, k1=k1: nc.tensor.matmul(
                                o_, Tt[r0:r0 + 64, :, k1], tabA[r0:r0 + 64, k1, :], start=True, stop=False),
                                reads=["Tt", "tabA"], writes=[f"ps:pG{b}"], signal=False)
                            kb.op("pe", lambda o_=o_, r0=r0, k1=k1: nc.tensor.matmul(
                                o_, Tt[r0:r0 + 64, :, 64 + k1], tabB[r0:r0 + 64, k1, :], start=False, stop=True),
                                reads=["Tt", "tabB"], writes=[f"ps:pG{b}"], signal=(k1 % 4 == 3))
                            if k1 % 4 == 3:
                                eng = "act" if ev % 2 == 0 else "dve"
                                ev += 1
                                k0 = k1 - 3
                                dst = X[:, c2, :, :].rearrange("p r (j k) -> p r k j", k=64)[:, :, k0:k0 + 4, :]
                                src = pG[b][:].rearrange("p (k r j) -> p r k j", k=4, r=2)
                                if eng == "act":
                                    kb.op("act", lambda dst=dst, src=src: nc.scalar.copy(dst, src), reads=[f"ps:pG{b}"], writes=["X"])
                                else:
                                    kb.op("dve", lambda dst=dst, src=src: nc.vector.tensor_copy(dst, src), reads=[f"ps:pG{b}"], writes=["X"])
                    for ocl in range(2):
                        og = fh * 2 + ocl
                        r0 = 64 * ocl
                        for kc in range(8):
                            b = kc % 2
                            n = 0
                            for c2 in range(2):
                                for ri in range(2):
                                    rhs = X[r0:r0 + 64, c2, ri, kc * 512:(kc + 1) * 512]
                                    kb.op("pe", lambda b=b, rhs=rhs, og=og, c2=c2, ri=ri, r0=r0, n=n: nc.tensor.matmul(
                                        pH[b][:], Mb[r0:r0 + 64, og * 4 + c2 * 2 + ri, :], rhs,
                                        start=(n == 0), stop=(n == 3)),
                                        reads=["X", "Mb"], writes=[f"ps:pH{b}"], signal=(n == 3))
                                    n += 1
                            eng = "act" if ev % 2 == 0 else "dve"
                            ev += 1
                            if eng == "act":
                                kb.op("act", lambda b=b: nc.scalar.copy(yst[b][:], pH[b][:]), reads=[f"ps:pH{b}"], writes=[f"yst{b}"])
                            else:
                                kb.op("dve", lambda b=b: nc.vector.tensor_copy(yst[b][:], pH[b][:]), reads=[f"ps:pH{b}"], writes=[f"yst{b}"])
                            kb.dma_ins("pool", lambda b=b, og=og, kc=kc: nc.gpsimd.indirect_dma_start(
                                out=yT_sh, out_offset=bass.IndirectOffsetOnAxis(ap=yidx[:, og:og + 1], axis=0),
                                in_=yst[b][:], in_offset=None, element_offset=kc * 512),
                                reads=[f"yst{b}", "yidx"], writes=[f"yT_sh{og}_{kc}"], sem=f"ys{b}")
                kb.barrier()
        if upto <= 3:
            if dbg:
                dump("yTsh", yT_sh, [8192, 512], BF16, [f"yT_sh{og}_{kc}" for og in range(2) for kc in range(8)])
            return finish()

        QT = big1[:, 0:16384].rearrange("p (h s) -> p h s", h=4)
        KT = big1[:, 16384:32768].rearrange("p (h s) -> p h s", h=4)
        SCALE = 96.0 ** -0.5
        with ExitStack() as sD:
            V_all = sb(sD, "V_all", [128, T, 2, 192], BF16)
            kb.op("dve", lambda: nc.vector.memset(V_all[:], 0.0), writes=["V_all"])
            kb.op("dve", lambda: nc.vector.memset(V_all[:, :, :, 64:65], 1.0), writes=["V_all"])
            with ExitStack() as sP:
                wqs = sb(sP, "wqs", [128, 3, 512], F32)
                wkvs = sb(sP, "wkvs", [128, 2, 512], F32)
                wq = sb(sP, "wq", [128, 3, 512], BF16)
                wkv = sb(sP, "wkv", [128, 2, 512], BF16)
                gq = sb(sP, "gq", [128, 3], F32)
                gkv = sb(sP, "gkv", [128, 2], F32)
                ut = [sb(sP, f"ut{i}", [128, 6, 128], BF16) for i in range(2)]
                Qt = [sb(sP, f"Qt{i}", [128, 4, 96], BF16) for i in range(2)]
                Kt = [sb(sP, f"Kt{i}", [128, 4, 96], BF16) for i in range(2)]
                cq = [sb(sP, f"cq{i}", [128, 32], F32) for i in range(2)]
                sq_ = [sb(sP, f"sq_{i}", [128, 32], F32) for i in range(2)]
                t1 = [sb(sP, f"t1{i}", [128, 4, 32], F32) for i in range(2)]
                t2 = [sb(sP, f"t2{i}", [128, 4, 32], F32) for i in range(2)]
                kr1 = [sb(sP, f"kr1{i}", [128, 32], F32) for i in range(2)]
                kr2 = [sb(sP, f"kr2{i}", [128, 32], F32) for i in range(2)]
                kro = [sb(sP, f"kro{i}", [128, 32], F32) for i in range(2)]
                pq = [ps(sP, f"pq{i}", [128, 512], F32) for i in range(2)]
                pkv = [ps(sP, f"pkv{i}", [128, 512], F32) for i in range(2)]
                pkr = ps(sP, "pkr", [128, 64], BF16)
                pqT = ps(sP, "pqT", [128, 4, 128], BF16)
                pkT = ps(sP, "pkT", [128, 4, 128], BF16)
                kb.dma("sp", wqs[:], A["w_q"].rearrange("(m p) n -> p m n", p=128), writes=["wqs"], sem="c3a")
                kb.dma("sp", wkvs[:], A["w_kv"].rearrange("(m p) n -> p m n", p=128), writes=["wkvs"], sem="c3b")
                kb.dma("sp", gq[:], A["g_q_lat"], writes=["gq"], sem="c3c")
                kb.dma("sp", gkv[:], A["g_kv_lat"], writes=["gkv"], sem="c3d")
                for m in range(3):
                    kb.op("pool", lambda m=m: nc.gpsimd.tensor_scalar(wq[:, m, :], wqs[:, m, :], gq[:, m:m + 1], None, ALU.mult),
                          reads=["wqs", "gq"], writes=["wq"])
                for m in range(2):
                    kb.op("pool", lambda m=m: nc.gpsimd.tensor_scalar(wkv[:, m, :], wkvs[:, m, :], gkv[:, m:m + 1], None, ALU.mult),
                          reads=["wkvs", "gkv"], writes=["wkv"])
                for t in range(T):
                    i = t % 2
                    tl = slice(t * 128, (t + 1) * 128)
                    kb.dma("sp", ut[i][:, 0:5, :], uT_d[0:5, :, tl].rearrange("m p s -> p m s"),
                           reads=["uT_d"], writes=[f"ut{i}"], sem=f"ut{i}")
                    kb.dma("sp", ut[i][0:64, 5, :], uT_d[5, 0:64, tl], reads=["uT_d"], writes=[f"ut{i}"], sem=f"ut{i}")
                    for m in range(3):
                        kb.op("pe", lambda m=m, i=i: nc.tensor.matmul(
                            pq[i][:], ut[i][:, m, :], wq[:, m, :], start=(m == 0), stop=(m == 2)),
                            reads=[f"ut{i}", "wq"], writes=[f"ps:pq{i}"], signal=(m == 2))
                    for m in range(2):
                        kb.op("pe", lambda m=m, i=i: nc.tensor.matmul(
                            pkv[i][:], ut[i][:, 3 + m, :], wkv[:, m, :], start=(m == 0), stop=(m == 1)),
                            reads=[f"ut{i}", "wkv"], writes=[f"ps:pkv{i}"], signal=(m == 1))
                    kb.op("pe", lambda i=i: nc.tensor.transpose(pkr[:], ut[i][0:64, 5, :], ident_bf[0:64, 0:64]),
                          reads=[f"ut{i}", "ident_bf"], writes=["ps:pkr"])
                    qv = pq[i][:, 0:384].rearrange("p (h d) -> p h d", d=96)
                    qs = pq[i][:, 384:512].rearrange("p (h d) -> p h d", d=32)
                    kb.op("act", lambda qv=qv, i=i, t=t: nc.scalar.activation(
                        Qt[i][:, :, 0:64], qv[:, :, 0:64], AF.Copy, scale=rq[:, t:t + 1]),
                        reads=[f"ps:pq{i}", "rq"], writes=[f"Qt{i}"])
                    kb.op("dve", lambda qv=qv, i=i, t=t: nc.vector.tensor_tensor(
                        t1[i][:], qv[:, :, 64:96], cos2[:, t, :].unsqueeze(1).to_broadcast([128, 4, 32]), ALU.mult),
                        reads=[f"ps:pq{i}", "rope_tab"], writes=[f"t1{i}"])
                    kb.op("dve", lambda qs=qs, i=i, t=t: nc.vector.scalar_tensor_tensor(
                        t2[i][:], qs, rq[:, t:t + 1], sin2s[:, t, :].unsqueeze(1).to_broadcast([128, 4, 32]), ALU.mult, ALU.mult),
                        reads=[f"ps:pq{i}", "rope_tab", "rq"], writes=[f"t2{i}"])
                    kb.op("dve", lambda i=i, t=t: nc.vector.scalar_tensor_tensor(
                        Qt[i][:, :, 64:96], t1[i][:], rq[:, t:t + 1], t2[i][:], ALU.mult, ALU.add),
                        reads=[f"t1{i}", f"t2{i}", "rq"], writes=[f"Qt{i}"])
                    kb.op("act", lambda i=i, t=t: nc.scalar.activation(
                        Kt[i][:, :, 0:64], pkv[i][:, 0:256].rearrange("p (h d) -> p h d", d=64), AF.Copy, scale=rkv[:, t:t + 1]),
                        reads=[f"ps:pkv{i}", "rkv"], writes=[f"Kt{i}"])
                    kb.op("dve", lambda t=t, i=i: nc.vector.tensor_tensor(kr1[i][:], pkr[:, 0:32], cos2[:, t, :], ALU.mult),
                          reads=["ps:pkr", "rope_tab"], writes=[f"kr1{i}"])
                    kb.op("dve", lambda t=t, i=i: nc.vector.tensor_tensor(kr2[i][:], pkr[:, 32:64], sin2s[:, t, :], ALU.mult),
                          reads=["ps:pkr", "rope_tab"], writes=[f"kr2{i}"])
                    kb.op("pool", lambda i=i: nc.gpsimd.tensor_tensor(kro[i][:], kr1[i][:], kr2[i][:], ALU.add),
                          reads=[f"kr1{i}", f"kr2{i}"], writes=[f"kro{i}"])
                    kb.op("pool", lambda i=i: nc.gpsimd.tensor_copy(
                        Kt[i][:, :, 64:96], kro[i][:].unsqueeze(1).to_broadcast([128, 4, 32])),
                        reads=[f"kro{i}"], writes=[f"Kt{i}"])
                    vv4 = pkv[i][:, 256:512].rearrange("p (a b d) -> p a b d", a=2, b=2)
                    kb.op("act", lambda t=t, vv4=vv4: nc.scalar.activation(
                        V_all[:, t, :, 0:64], vv4[:, :, 0, :], AF.Copy, scale=rkv[:, t:t + 1]),
                        reads=[f"ps:pkv{i}", "rkv"], writes=["V_all"])
                    kb.op("dve", lambda t=t, vv4=vv4: nc.vector.tensor_scalar(
                        V_all[:, t, :, 128:192], vv4[:, :, 1, :], rkv[:, t:t + 1], None, ALU.mult),
                        reads=[f"ps:pkv{i}", "rkv"], writes=["V_all"])
                    for h in range(4):
                        kb.op("pe", lambda h=h, i=i: nc.tensor.transpose(pqT[0:96, h, :], Qt[i][:, h, :], ident_bf[:]),
                              reads=[f"Qt{i}", "ident_bf"], writes=["ps:pqT"], signal=(h == 3))
                    for h in range(4):
                        kb.op("pe", lambda h=h, i=i: nc.tensor.transpose(pkT[0:96, h, :], Kt[i][:, h, :], ident_bf[:]),
                              reads=[f"Kt{i}", "ident_bf"], writes=["ps:pkT"], signal=(h == 3))
                    kb.op("act", lambda tl=tl: nc.scalar.copy(QT[0:96, :, tl], pqT[0:96, :, :]), reads=["ps:pqT"], writes=["QT"])
                    kb.op("dve", lambda tl=tl: nc.vector.tensor_copy(KT[0:96, :, tl], pkT[0:96, :, :]), reads=["ps:pkT"], writes=["KT"])
                kb.barrier()
            if upto <= 4:
                if dbg:
                    dump("QT", big1[0:96, 0:16384], [96, 16384], BF16, ["QT"])
                    dump("KT", big1[0:96, 16384:32768], [96, 16384], BF16, ["KT"])
                    dump("V", V_all[:], [128, T, 2, 192], BF16, ["V_all"])
                return finish()
            with ExitStack() as sM:
                NB = 4
                PT = [sb(sM, f"PT{i}", [128, 512], BF16) for i in range(NB)]
                recs = sb(sM, "recs", [128, 512], F32)
                bcs = sb(sM, "bcs", [128, 512], F32)
                ysa = [sb(sM, f"ysa{i}", [128, 512], BF16) for i in range(2)]
                pS = [ps(sM, f"pS{i}", [128, 512], F32) for i in range(NB)]
                pO = [ps(sM, f"pO{i}", [128, 512], F32) for i in range(2)]
                pbc = ps(sM, "pbc", [128, 512], F32)
                ngroups = 1
                nqc = int(os.environ.get("KNQC", "8"))
                yi = 0
                for g2 in range(ngroups):
                    if g2 == 1:
                        kb.barrier()
                        kb.dma("sp", QT[0:96, :, :], qk_d[0], reads=["qk_d"], writes=["QT"], sem="c4")
                        kb.dma("sp", KT[0:96, :, :], qk_d[1], reads=["qk_d"], writes=["KT"], sem="c4")
                    for pair in range(2):
                        gp = 2 * g2 + pair
                        for qc in range(nqc):
                            qsl = slice(qc * 512, (qc + 1) * 512)
                            yb = yi % 2
                            yi += 1
                            for hh in range(2):
                                hl = 2 * pair + hh
                                O = pO[hh]
                                if hh == 0:
                                    lv = lambda kt: V_all[:, kt, gp, 0:65]
                                    Mo = 65
                                else:
                                    lv = lambda kt: V_all[:, kt, gp, 64:192]
                                    Mo = 128

                                def smm(kt, hl=hl, qsl=qsl):
                                    b = kt % NB
                                    kb.op("pe", lambda b=b, kt=kt: nc.tensor.matmul(
                                        pS[b][:], KT[0:96, hl, kt * 128:(kt + 1) * 128], QT[0:96, hl, qsl],
                                        start=True, stop=True),
                                        reads=["QT", "KT"], writes=[f"ps:pS{b}"])
                                smm(0)
                                smm(1)
                                for kt in range(T):
                                    b = kt % NB
                                    if kt + 2 < T:
                                        smm(kt + 2)
                                    kb.op("act", lambda b=b: nc.scalar.activation(PT[b][:], pS[b][:], AF.Exp, scale=SCALE),
                                          reads=[f"ps:pS{b}"], writes=[f"PT{b}"])
                                    kb.op("pe", lambda b=b, kt=kt, O=O, lv=lv, Mo=Mo: nc.tensor.matmul(
                                        O[0:Mo, :], lv(kt), PT[b][:], start=(kt == 0), stop=(kt == T - 1)),
                                        reads=[f"PT{b}", "V_all"], writes=[f"ps:pO{hh}"], signal=(kt == T - 1))
                                if hh == 0:
                                    kb.op("dve", lambda O=O: nc.vector.reciprocal(recs[64:65, :], O[64:65, :]),
                                          reads=["ps:pO0"], writes=["recs"])
                                    kb.op("pe", lambda: nc.tensor.matmul(pbc[0:64, :], ones_f[64:65, 0:64], recs[64:65, :],
                                                                         start=True, stop=True),
                                          reads=["recs", "ones_f"], writes=["ps:pbc"])
                                    kb.op("act", lambda: nc.scalar.copy(bcs[0:64, :], pbc[0:64, :]), reads=["ps:pbc"], writes=["bcs"])
                                    kb.op("dve", lambda O=O, yb=yb: nc.vector.tensor_tensor(ysa[yb][0:64, :], O[0:64, :], bcs[0:64, :], ALU.mult),
                                          reads=["ps:pO0", "bcs"], writes=[f"ysa{yb}"])
                                else:
                                    kb.op("dve", lambda O=O: nc.vector.reciprocal(recs[0:1, :], O[0:1, :]),
                                          reads=["ps:pO1"], writes=["recs"])
                                    kb.op("pe", lambda: nc.tensor.matmul(pbc[:, :], ones_f[0:1, :], recs[0:1, :],
                                                                         start=True, stop=True),
                                          reads=["recs", "ones_f"], writes=["ps:pbc"])
                                    kb.op("act", lambda: nc.scalar.copy(bcs[64:128, :], pbc[64:128, :]), reads=["ps:pbc"], writes=["bcs"])
                                    kb.op("dve", lambda O=O, yb=yb: nc.vector.tensor_tensor(ysa[yb][64:128, :], O[64:128, :], bcs[64:128, :], ALU.mult),
                                          reads=["ps:pO1", "bcs"], writes=[f"ysa{yb}"])
                            kb.dma_ins("pool", lambda yb=yb, gp=gp, qc=qc: nc.gpsimd.indirect_dma_start(
                                out=yT_sh, out_offset=bass.IndirectOffsetOnAxis(ap=yidx[:, 2 + gp:3 + gp], axis=0),
                                in_=ysa[yb][:], in_offset=None, element_offset=qc * 512),
                                reads=[f"ysa{yb}", "yidx"], writes=[f"yT_sha{gp}_{qc}"], sem=f"ya{yb}")
                kb.barrier()
        if upto <= 5:
            if dbg:
                dump("yTsh", yT_sh, [8192, 512], BF16, ["uT_d"])
            return finish()

        sAD.close()
        pair_barrier(0)
        h3rows = big1[:, 0:HT * ROW].rearrange("p (t r) -> p t r", t=HT)

        def rstd_of(col, t, n):
            kb.op("act", lambda: nc.scalar.activation(rsall[:, col, t:t + 1], ssall[:, col, t:t + 1], AF.Sqrt,
                                                      bias=EPS, scale=1.0 / n),
                  reads=[f"ss{col}_{t}"], writes=[f"rs{col}_{t}"])
            kb.op("dve", lambda: nc.vector.reciprocal(rsall[:, col, t:t + 1], rsall[:, col, t:t + 1]),
                  reads=[f"rs{col}_{t}"], writes=[f"rs{col}_{t}"])

        with ExitStack() as sE:
            wo = sb(sE, "wo", [128, 8, D], BF16)
            wmq = sb(sE, "wmq", [128, 8, D], BF16)
            wmo = sb(sE, "wmo", [128, 8, D], BF16)
            wr = sb(sE, "wr", [128, 8, NE], F32)
            gmq = sb(sE, "gmq", [128, D], F32)
            gffn = sb(sE, "gffn", [128, D], F32)
            tokid = sb(sE, "tokid", [128, HT], I32)
            gidx = sb(sE, "gidx", [128, 8], I32)
            x2idx = sb(sE, "x2idx", [128, HT], I32)
            zidx = sb(sE, "zidx", [128, HT], I32)
            hidx = sb(sE, "hidx", [128, 1], I32)
            aidx = sb(sE, "aidx", [128, 2], I32)
            zt = sb(sE, "zt", [128, D], F32)
            for nm_, t_ in (("gidx", gidx), ("x2idx", x2idx), ("zidx", zidx), ("hidx", hidx), ("aidx", aidx)):
                kb.dma("sp", t_[:], A[nm_], writes=[nm_], sem="c5")
            kb.join(["gidx", "x2idx", "zidx", "hidx", "aidx"], "c5")
            kb.op("pool", lambda: nc.gpsimd.memset(zt[:], 0.0), writes=["zt"])
            for tl_ in range(HT):
                kb.dma_ins("pool", lambda tl_=tl_: nc.gpsimd.indirect_dma_start(
                    out=accp_sh, out_offset=bass.IndirectOffsetOnAxis(ap=zidx[:, tl_:tl_ + 1], axis=0),
                    in_=zt[:], in_offset=None), reads=["zt", "zidx"], writes=[f"accz{tl_}"], sem="az")
            kmT = sb(sE, "kmT", [128, 8, 256], BF16)
            vm = sb(sE, "vm", [128, 2, D], BF16)
            kb.dma("sp", wr[:], A["w_router"].rearrange("(c p) e -> p c e", p=128), writes=["wr"], sem="c5b")
            kb.dma("sp", gmq[:], A["g_mem_q"], writes=["gmq"], sem="c5b")
            kb.dma("sp", gffn[:], A["g_ffn"], writes=["gffn"], sem="c5b")
            kb.dma("sp", tokid[:], A["tokid"], writes=["tokid"], sem="c5b")
            kb.join(["wr", "gmq", "gffn", "tokid"], "c5b")
            kb.op("dve", lambda: nc.vector.memset(big1[:, 0:HT * ROW], 0.0), writes=["h3rows"])
            kb.op("pool", lambda: nc.gpsimd.tensor_copy(
                h3rows[:, :, 1056:1058].bitcast(I32), tokid[:].unsqueeze(2)), reads=["tokid"], writes=["h3rows"])
            with ExitStack() as sK:
                wmkv = sb(sK, "wmkv", [128, 8, 2048], BF16)
                gmkv = sb(sK, "gmkv", [128, D], F32)
                mt_ = sb(sK, "mt_", [128, D], F32)
                mb_ = sb(sK, "mb_", [128, D], BF16)
                sqm = sb(sK, "sqm", [128, D], BF16)
                mnT = sb(sK, "mnT", [128, 8, 256], BF16)
                pT = ps(sK, "pTm", [128, 8, 128], BF16)
                pA = [ps(sK, f"pAm{i}", [128, 512], F32) for i in range(2)]
                for c in range(8):
                    kb.dma("pool", wmkv[:, c, :], A["w_mem_kv"][c * 128:(c + 1) * 128, :], writes=[f"wmkv_{c}"], sem="wk")
                kb.join(["wmkv"], "wk")
                for c in range(8):
                    cs = slice(c * 128, (c + 1) * 128)
                    kb.dma("pool", wo[:, c, :], A["w_out"][cs, :], writes=[f"wo_{c}"], sem="we")
                    kb.dma("pool", wmq[:, c, :], A["w_mem_q"][cs, :], writes=[f"wmq_{c}"], sem="we")
                    kb.dma("pool", wmo[:, c, :], A["w_mem_o"][cs, :], writes=[f"wmo_{c}"], sem="we")
                kb.join(["wo", "wmq", "wmo"], "we")
                kb.dma("sp", gmkv[:], A["g_mem_kv"], writes=["gmkv"], sem="c5g")
                for mt in range(2):
                    kb.dma("sp", mt_[:], A["mem"][mt * 128:(mt + 1) * 128, :], writes=["mt_"], sem="c5m")
                    kb.op("act", lambda mt=mt: nc.scalar.activation(sqm[:], mt_[:], AF.Square, accum_out=ssall[:, 4, mt:mt + 1]),
                          reads=["mt_"], writes=["sqm", f"ss4_{mt}"])
                    rstd_of(4, mt, D)
                    kb.op("dve", lambda mt=mt: nc.vector.scalar_tensor_tensor(
                        mb_[:], mt_[:], rsall[:, 4, mt:mt + 1], gmkv[:], ALU.mult, ALU.mult),
                        reads=["mt_", f"rs4_{mt}", "gmkv"], writes=["mb_"])
                    for c in range(8):
                        kb.op("pe", lambda c=c: nc.tensor.transpose(pT[:, c, :], mb_[:, c * 128:(c + 1) * 128], ident_bf[:]),
                              reads=["mb_", "ident_bf"], writes=["ps:pTm"], signal=(c == 7))
                    kb.op("act", lambda mt=mt: nc.scalar.copy(mnT[:, :, mt * 128:(mt + 1) * 128], pT[:]),
                          reads=["ps:pTm"], writes=["mnT"])
                n_ = 0
                for hc in range(8):
                    b = n_ % 2
                    n_ += 1
                    for c in range(8):
                        kb.op("pe", lambda b=b, hc=hc, c=c: nc.tensor.matmul(
                            pA[b][:, 0:256], wmkv[:, c, hc * 128:(hc + 1) * 128], mnT[:, c, :], start=(c == 0), stop=(c == 7)),
                            reads=["wmkv", "mnT"], writes=[f"ps:pAm{b}"], signal=(c == 7))
                    kb.op("dve", lambda b=b, hc=hc: nc.vector.tensor_copy(kmT[:, hc, :], pA[b][:, 0:256]),
                          reads=[f"ps:pAm{b}"], writes=["kmT"])
                for mt in range(2):
                    for half in range(2):
                        b = n_ % 2
                        n_ += 1
                        for c in range(8):
                            kb.op("pe", lambda b=b, mt=mt, half=half, c=c: nc.tensor.matmul(
                                pA[b][:], mnT[:, c, mt * 128:(mt + 1) * 128],
                                wmkv[:, c, 1024 + half * 512:1024 + (half + 1) * 512], start=(c == 0), stop=(c == 7)),
                                reads=["wmkv", "mnT"], writes=[f"ps:pAm{b}"], signal=(c == 7))
                        kb.op("act", lambda b=b, mt=mt, half=half: nc.scalar.copy(vm[:, mt, half * 512:(half + 1) * 512], pA[b][:]),
                              reads=[f"ps:pAm{b}"], writes=["vm"])
                kb.barrier()
            with ExitStack() as sL:
                UP = 17408
                yt = [sb(sL, "yt0", [128, 8, 512], BF16), big1[:, UP:UP + 4096].rearrange("p (c s) -> p c s", c=8)]
                qmT = big1[:, UP + 4096:UP + 8192].rearrange("p (c s) -> p c s", c=8)
                OT = big1[:, UP + 8192:UP + 12288].rearrange("p (c s) -> p c s", c=8)
                xt = [sb(sL, f"xe{i}", [128, D], F32) for i in range(2)]
                x1s = sb(sL, "x1s", [128, 4, D], F32)
                hb = [sb(sL, f"hbe{i}", [128, D], BF16) for i in range(2)]
                sqj = sb(sL, "sqe", [128, D], BF16)
                h2T = sb(sL, "h2T", [128, 8, 512], BF16)
                PTm = [sb(sL, f"PTm{i}", [128, 512], BF16) for i in range(2)]
                recm = sb(sL, "recm", [128, 512], F32)
                h3f = [sb(sL, f"h3f{i}", [128, D], F32) for i in range(2)]
                h3T = sb(sL, "h3T", [128, 8, 128], F32)
                lg = sb(sL, "lg", [128, NE], F32)
                ex = sb(sL, "ex", [128, NE], F32)
                sm = sb(sL, "sm", [128, 4], F32)
                pX = [ps(sL, f"pX{i}", [128, 512], F32) for i in range(2)]
                pT = ps(sL, "pTe", [128, 8, 128], BF16)
                pA = [ps(sL, f"pAe{i}", [128, 512], F32) for i in range(2)]
                pO = [ps(sL, f"pOe{i}", [128, 512], F32) for i in range(2)]
                pSm = ps(sL, "pSm", [128, 512], F32)
                MSC = 256.0 ** -0.5
                nst = 4

                def load_yt(n):
                    yb = n % 2
                    war = list(kb.readers.get(f"yt{yb}", []))
                    tok = None
                    for c in range(8):
                        tok = kb.dma_ins("pool", lambda c=c, n=n, yb=yb: nc.gpsimd.indirect_dma_start(
                            out=yt[yb][:, c, :], out_offset=None, in_=yT_sh,
                            in_offset=bass.IndirectOffsetOnAxis(ap=gidx[:, c:c + 1], axis=0), element_offset=n * 512),
                            reads=["gidx"], writes=[], sem=f"yt{yb}", extra=war)
                    kb.lastw[f"yt{yb}"] = tok
                    kb.readers[f"yt{yb}"] = []

                def s1_front(n, j):
                    t = 4 * n + j
                    i = t % 2
                    yb = n % 2
                    js = slice(j * 128, (j + 1) * 128)
                    kb.dma("sp", xt[i][:], A["xh"][t * 128:(t + 1) * 128, :], writes=[f"xe{i}"], sem=f"xe{i}")
                    for half in range(2):
                        hs = slice(half * 512, (half + 1) * 512)
                        for c in range(8):
                            kb.op("pe", lambda half=half, c=c, js=js, hs=hs, yb=yb: nc.tensor.matmul(
                                pX[half][:], yt[yb][:, c, js], wo[:, c, hs], start=(c == 0), stop=(c == 7)),
                                reads=[f"yt{yb}", "wo"], writes=[f"ps:pX{half}"], signal=(c == 7))
                        kb.op("dve", lambda half=half, j=j, hs=hs, i=i: nc.vector.tensor_tensor(
                            x1s[:, j, hs], pX[half][:], xt[i][:, hs], ALU.add),
                            reads=[f"ps:pX{half}", f"xe{i}"], writes=[f"x1s{j}"])
                    kb.op("act", lambda j=j, t=t: nc.scalar.activation(sqj[:], x1s[:, j, :], AF.Square,
                                                                       accum_out=ssall[:, 1, t:t + 1]),
                          reads=[f"x1s{j}"], writes=["sqe", f"ss1_{t}"])
                    rstd_of(1, t, D)
                    kb.op("dve", lambda j=j, t=t, i=i: nc.vector.scalar_tensor_tensor(
                        hb[i][:], x1s[:, j, :], rsall[:, 1, t:t + 1], gmq[:], ALU.mult, ALU.mult),
                        reads=[f"x1s{j}", f"rs1_{t}", "gmq"], writes=[f"hbe{i}"])

                def s1_back(n, j):
                    t = 4 * n + j
                    i = t % 2
                    js = slice(j * 128, (j + 1) * 128)
                    for c in range(8):
                        kb.op("pe", lambda c=c, i=i: nc.tensor.transpose(pT[:, c, :], hb[i][:, c * 128:(c + 1) * 128], ident_bf[:]),
                              reads=[f"hbe{i}", "ident_bf"], writes=["ps:pTe"], signal=(c == 7))
                    if j % 2 == 0:
                        kb.op("act", lambda js=js: nc.scalar.copy(h2T[:, :, js], pT[:]), reads=["ps:pTe"], writes=["h2T"])
                    else:
                        kb.op("dve", lambda js=js: nc.vector.tensor_copy(h2T[:, :, js], pT[:]), reads=["ps:pTe"], writes=["h2T"])

                def s3_front(n, j):
                    t = 4 * n + j
                    i = j
                    k = t % 2
                    js = slice(j * 128, (j + 1) * 128)
                    for half in range(2):
                        hs = slice(half * 512, (half + 1) * 512)
                        for c in range(8):
                            kb.op("pe", lambda half=half, c=c, js=js, hs=hs: nc.tensor.matmul(
                                pX[half][:], OT[:, c, js], wmo[:, c, hs], start=(c == 0), stop=(c == 7)),
                                reads=["OT", "wmo"], writes=[f"ps:pX{half}"], signal=(c == 7))
                        kb.op("dve", lambda half=half, j=j, hs=hs: nc.vector.tensor_tensor(
                            x1s[:, j, hs], pX[half][:], x1s[:, j, hs], ALU.add),
                            reads=[f"ps:pX{half}", f"x1s{j}"], writes=[f"x1s{j}"])
                    kb.dma_ins("pool", lambda t=t, j=j: nc.gpsimd.indirect_dma_start(
                        out=accp_sh, out_offset=bass.IndirectOffsetOnAxis(ap=x2idx[:, t:t + 1], axis=0),
                        in_=x1s[:, j, :], in_offset=None), reads=[f"x1s{j}", "x2idx"], writes=[f"accx{t}"], sem=f"x2{i}")
                    kb.op("act", lambda j=j, t=t: nc.scalar.activation(sqj[:], x1s[:, j, :], AF.Square,
                                                                       accum_out=ssall[:, 2, t:t + 1]),
                          reads=[f"x1s{j}"], writes=["sqe", f"ss2_{t}"])
                    rstd_of(2, t, D)
                    kb.op("dve", lambda j=j, t=t, k=k: nc.vector.scalar_tensor_tensor(
                        h3f[k][:], x1s[:, j, :], rsall[:, 2, t:t + 1], gffn[:], ALU.mult, ALU.mult),
                        reads=[f"x1s{j}", f"rs2_{t}", "gffn"], writes=[f"h3f{k}"])
                    kb.op("pool", lambda t=t, k=k: nc.gpsimd.tensor_copy(h3rows[:, t, 0:D], h3f[k][:]),
                          reads=[f"h3f{k}"], writes=["h3rows"])

                def s3_back(n, j):
                    t = 4 * n + j
                    k = t % 2
                    for half in range(2):
                        pXv = pA[half][:].rearrange("p (c k) -> p c k", c=4)
                        for c4 in range(4):
                            c = half * 4 + c4
                            kb.op("pe", lambda pXv=pXv, c4=c4, c=c, k=k: nc.tensor.transpose(
                                pXv[:, c4, :], h3f[k][:, c * 128:(c + 1) * 128], ident_f[:]),
                                reads=[f"h3f{k}", "ident_f"], writes=[f"ps:pAe{half}"], signal=(c4 == 3))
                        if half == 0:
                            kb.op("act", lambda pXv=pXv: nc.scalar.copy(h3T[:, 0:4, :], pXv), reads=["ps:pAe0"], writes=["h3T"])
                        else:
                            kb.op("dve", lambda pXv=pXv: nc.vector.tensor_copy(h3T[:, 4:8, :], pXv), reads=["ps:pAe1"], writes=["h3T"])
                    for c in range(8):
                        kb.op("pe", lambda c=c: nc.tensor.matmul(pSm[:, 0:NE], h3T[:, c, :], wr[:, c, :], start=(c == 0), stop=(c == 7)),
                              reads=["h3T", "wr"], writes=["ps:pSm"], signal=(c == 7))
                    kb.op("dve", lambda: nc.vector.tensor_copy(lg[:], pSm[:, 0:NE]), reads=["ps:pSm"], writes=["lg"])
                    kb.op("dve", lambda: nc.vector.tensor_reduce(sm[:, 0:1], lg[:], AX.X, ALU.max), reads=["lg"], writes=["sm0"])
                    kb.op("dve", lambda: nc.vector.tensor_scalar(sm[:, 1:2], sm[:, 0:1], -1.0, None, ALU.mult), reads=["sm0"], writes=["sm1"])
                    kb.op("act", lambda t=t: nc.scalar.activation(ex[:], lg[:], AF.Exp, bias=sm[:, 1:2], accum_out=ssall[:, 3, t:t + 1]),
                          reads=["lg", "sm1"], writes=["ex", f"ss3_{t}"])
                    kb.op("dve", lambda t=t: nc.vector.reciprocal(sm[:, 2:3], ssall[:, 3, t:t + 1]), reads=[f"ss3_{t}"], writes=["sm2"])
                    kb.op("dve", lambda t=t: nc.vector.tensor_scalar(affl[:, t, :], ex[:], sm[:, 2:3], None, ALU.mult),
                          reads=["ex", "sm2"], writes=["affl"])
                    kb.op("pool", lambda t=t: nc.gpsimd.tensor_copy(h3rows[:, t, 1024:1056].bitcast(F32), affl[:, t, :]),
                          reads=["affl"], writes=["h3rows"])

                load_yt(0)
                for n in range(nst):
                    s1_front(n, 0)
                    for j in range(4):
                        if j + 1 < 4:
                            s1_front(n, j + 1)
                        s1_back(n, j)
                    if n + 1 < nst:
                        load_yt(n + 1)
                    for hc in range(8):
                        b = hc % 2
                        for c in range(8):
                            kb.op("pe", lambda b=b, hc=hc, c=c: nc.tensor.matmul(
                                pA[b][:], wmq[:, c, hc * 128:(hc + 1) * 128], h2T[:, c, :], start=(c == 0), stop=(c == 7)),
                                reads=["wmq", "h2T"], writes=[f"ps:pAe{b}"], signal=(c == 7))
                        if hc % 2 == 0:
                            kb.op("act", lambda b=b, hc=hc: nc.scalar.copy(qmT[:, hc, :], pA[b][:]), reads=[f"ps:pAe{b}"], writes=["qmT"])
                        else:
                            kb.op("dve", lambda b=b, hc=hc: nc.vector.tensor_copy(qmT[:, hc, :], pA[b][:]), reads=[f"ps:pAe{b}"], writes=["qmT"])
                    for hh in range(4):
                        for mt in range(2):
                            for dc in range(2):
                                kb.op("pe", lambda mt=mt, dc=dc, hh=hh: nc.tensor.matmul(
                                    pA[mt][0:128, :], kmT[:, hh * 2 + dc, mt * 128:(mt + 1) * 128], qmT[:, hh * 2 + dc, :],
                                    start=(dc == 0), stop=(dc == 1)),
                                    reads=["kmT", "qmT"], writes=[f"ps:pAe{mt}"], signal=(dc == 1))
                            kb.op("act", lambda mt=mt: nc.scalar.activation(PTm[mt][:], pA[mt][:], AF.Exp, scale=MSC),
                                  reads=[f"ps:pAe{mt}"], writes=[f"PTm{mt}"])
                        for mt in range(2):
                            kb.op("pe", lambda mt=mt: nc.tensor.matmul(pSm[:], ones_bf[:], PTm[mt][:], start=(mt == 0), stop=(mt == 1)),
                                  reads=["ones_bf", f"PTm{mt}"], writes=["ps:pSm"], signal=(mt == 1))
                        for dc in range(2):
                            for mt in range(2):
                                kb.op("pe", lambda mt=mt, dc=dc, hh=hh: nc.tensor.matmul(
                                    pO[dc][:], vm[:, mt, hh * 256 + dc * 128:hh * 256 + (dc + 1) * 128], PTm[mt][:],
                                    start=(mt == 0), stop=(mt == 1)),
                                    reads=["vm", f"PTm{mt}"], writes=[f"ps:pOe{dc}"], signal=(mt == 1))
                        kb.op("dve", lambda: nc.vector.reciprocal(recm[:], pSm[:]), reads=["ps:pSm"], writes=["recm"])
                        for dc in range(2):
                            kb.op("dve", lambda dc=dc, hh=hh: nc.vector.tensor_tensor(OT[:, hh * 2 + dc, :], pO[dc][:], recm[:], ALU.mult),
                                  reads=[f"ps:pOe{dc}", "recm"], writes=["OT"])
                    s3_front(n, 0)
                    for j in range(4):
                        if j + 1 < 4:
                            s3_front(n, j + 1)
                        s3_back(n, j)
                kb.dma_ins("pool", lambda: nc.gpsimd.indirect_dma_start(
                    out=affs_sh, out_offset=bass.IndirectOffsetOnAxis(ap=hidx[:, 0:1], axis=0),
                    in_=affl[:].rearrange("p t e -> p (t e)"), in_offset=None),
                    reads=["affl", "hidx"], writes=["affs_sh"], sem="afs")
                pair_barrier(1)
                for hh_ in range(2):
                    kb.dma_ins("pool", lambda hh_=hh_: nc.gpsimd.indirect_dma_start(
                        out=affs[:, hh_ * HT:(hh_ + 1) * HT, :].rearrange("p t e -> p (t e)"), out_offset=None, in_=affs_sh,
                        in_offset=bass.IndirectOffsetOnAxis(ap=aidx[:, hh_:hh_ + 1], axis=0)),
                        reads=["aidx", "affs_sh"], writes=["affs"], sem="afg")
                kb.barrier()
        if upto <= 6:
            if dbg:
                dump("affs", affs[:], [128, T, NE], F32, ["affs"])
                dump("h3rows", big1[:, 0:HT * ROW], [128, HT * ROW], BF16, ["h3rows"])
                dump("accp", accp_sh, [2 * S, D], F32, ["affs"])
            return finish()

        with ExitStack() as sT:
            lo = sb(sT, "lo", [128, NE], F32)
            mid = sb(sT, "mid", [128, NE], F32)
            dlt = sb(sT, "dlt", [128, NE], F32)
            cnt = sb(sT, "cnt", [128, NE], F32)
            msk = sb(sT, "msk", [128, T, NE], F32)
            sel = sb(sT, "sel", [128, T, NE], BF16)
            tot = sb(sT, "tot", [128, T, NE], F32)
            incl = sb(sT, "incl", [128, NE, HT], F32)
            posf = sb(sT, "posf2", [128, HT, NE], F32)
            pen = sb(sT, "pen", [128, HT, NE], F32)
            toth = sb(sT, "toth", [128, NE], F32)
            eoff = sb(sT, "eoff", [128, HT, NE], F32)
            kb.dma("sp", eoff[:], A["eoff"], writes=["eoff"], sem="c7")
            pC = ps(sT, "pC", [128, 512], F32)
            pP = ps(sT, "pP", [128, 512], F32)
            pQ = ps(sT, "pQ", [128, 512], F32)
            kb.op("dve", lambda: nc.vector.memset(lo[:], 0.0), writes=["lo"])
            for k in range(34):
                w2 = 1.5 / (2.0 ** (k + 1))
                kb.op("dve", lambda w2=w2: nc.vector.tensor_scalar(mid[:], lo[:], w2, None, ALU.add), reads=["lo"], writes=["mid"])
                kb.op("dve", lambda: nc.vector.tensor_tensor(
                    msk[:], affs[:], mid[:].unsqueeze(1).to_broadcast([128, T, NE]), ALU.is_ge),
                    reads=["affs", "mid"], writes=["msk"])
                kb.op("dve", lambda: nc.vector.tensor_reduce(cnt[:], msk[:].rearrange("p t e -> p e t"), AX.X, ALU.add),
                      reads=["msk"], writes=["cnt"])
                kb.op("pe", lambda: nc.tensor.matmul(pC[:, 0:NE], ones_f[:], cnt[:], start=True, stop=True),
                      reads=["ones_f", "cnt"], writes=["ps:pC"])
                kb.op("dve", lambda w2=w2: nc.vector.tensor_scalar(dlt[:], pC[:, 0:NE], CAP - 0.5, w2, ALU.is_ge, ALU.mult),
                      reads=["ps:pC"], writes=["dlt"])
                kb.op("dve", lambda: nc.vector.tensor_tensor(lo[:], lo[:], dlt[:], ALU.add), reads=["lo", "dlt"], writes=["lo"])
            kb.op("dve", lambda: nc.vector.tensor_tensor(
                sel[:], affs[:], lo[:].unsqueeze(1).to_broadcast([128, T, NE]), ALU.is_ge),
                reads=["affs", "lo"], writes=["sel"])
            self_flat = sel[:].rearrange("p t e -> p (t e)")
            own_flat = sel[:, 0:HT, :].rearrange("p t e -> p (t e)")
            kb.op("pe", lambda: nc.tensor.matmul(pP[:, 0:HT * NE], ustrict[:], own_flat, start=True, stop=True),
                  reads=["ustrict", "sel"], writes=["ps:pP"])
            kb.op("pe", lambda: nc.tensor.matmul(pQ[:], ones_bf[:], self_flat, start=True, stop=True),
                  reads=["ones_bf", "sel"], writes=["ps:pQ"])
            kb.op("dve", lambda: nc.vector.tensor_copy(tot[:].rearrange("p t e -> p (t e)"), pQ[:]), reads=["ps:pQ"], writes=["tot"])
            for e in range(NE):
                kb.op("dve", lambda e=e: nc.vector.tensor_tensor_scan(
                    incl[:, e, :], ones_f[:, 0:HT], tot[:, 0:HT, e], 0.0, ALU.mult, ALU.add),
                    reads=["tot", "ones_f"], writes=["incl"])
            kb.op("dve", lambda: nc.vector.tensor_reduce(toth[:], tot[:, HT:T, :].rearrange("p t e -> p e t"), AX.X, ALU.add),
                  reads=["tot"], writes=["toth"])
            kb.op("dve", lambda: nc.vector.tensor_scalar(toth[:], toth[:], jf[:, 0:1], None, ALU.mult), reads=["toth", "jf"], writes=["toth"])
            kb.op("dve", lambda: nc.vector.tensor_tensor(posf[:], incl[:].rearrange("p e t -> p t e"), tot[:, 0:HT, :], ALU.subtract),
                  reads=["incl", "tot"], writes=["posf2"])
            kb.op("dve", lambda: nc.vector.tensor_tensor(posf[:].rearrange("p t e -> p (t e)"), pP[:, 0:HT * NE],
                                                         posf[:].rearrange("p t e -> p (t e)"), ALU.add),
                  reads=["ps:pP", "posf2"], writes=["posf2"])
            kb.op("dve", lambda: nc.vector.tensor_tensor(posf[:], posf[:], toth[:].unsqueeze(1).to_broadcast([128, HT, NE]), ALU.add),
                  reads=["posf2", "toth"], writes=["posf2"])
            kb.op("dve", lambda: nc.vector.tensor_scalar(pen[:], posf[:], CAP - 0.5, BIG, ALU.is_ge, ALU.mult), reads=["posf2"], writes=["pen"])
            kb.op("dve", lambda: nc.vector.tensor_tensor(posf[:], posf[:], pen[:], ALU.add), reads=["posf2", "pen"], writes=["posf2"])
            kb.op("dve", lambda: nc.vector.tensor_scalar(pen[:], sel[:, 0:HT, :], -BIG, BIG, ALU.mult, ALU.add), reads=["sel"], writes=["pen"])
            kb.op("dve", lambda: nc.vector.tensor_tensor(posf[:], posf[:], pen[:], ALU.add), reads=["posf2", "pen"], writes=["posf2"])
            kb.op("dve", lambda: nc.vector.tensor_tensor(posf[:], posf[:], eoff[:], ALU.add), reads=["posf2", "eoff"], writes=["posf2"])
            kb.op("dve", lambda: nc.vector.tensor_copy(posi[:], posf[:]), reads=["posf2"], writes=["posi"])
            kb.barrier()
        if upto <= 7:
            if dbg:
                dump("posi", posi[:], [128, HT, NE], I32, ["posi"])
            return finish()

        with ExitStack() as sX:
            wg = [sb(sX, f"wg{i}", [128, 8, D], BF16) for i in range(2)]
            wu = [sb(sX, f"wu{i}", [128, 8, D], BF16) for i in range(2)]
            wd = sb(sX, "wd", [128, 8, D], BF16)
            xsb = [sb(sX, f"xs{i}", [128, 4, ROW], BF16) for i in range(2)]
            xinT = sb(sX, "xinT", [128, 8, 512], BF16)
            hTe = sb(sX, "hTe", [128, 8, 512], BF16)
            sa = sb(sX, "sa", [128, 512], F32)
            ysb = sb(sX, "ysb", [128, D], F32)
            pT = ps(sX, "pTx", [128, 8, 128], BF16)
            pA = [ps(sX, f"pGa{i}", [128, 512], F32) for i in range(2)]
            pB_ = [ps(sX, f"pGb{i}", [128, 512], F32) for i in range(2)]
            pY = [ps(sX, f"pY{i}", [128, 512], F32) for i in range(2)]
            nexp = 8
            xidx = sb(sX, "xidx", [128, 32], I32)
            gt = sb(sX, "gt", [128, 8], F32)
            idf = sb(sX, "idf", [128, 4], F32)
            idt = sb(sX, "idt", [128, 4], I32)
            kb.dma("sp", xidx[:], A["xidx"], writes=["xidx"], sem="c8")
            reg_cap = nc.gpsimd.alloc_register("reg_cap")
            nc.gpsimd.reg_mov(reg_cap, NE * CAP - 1)
            reg_s = nc.gpsimd.alloc_register("reg_s")
            nc.gpsimd.reg_mov(reg_s, 2 * S - 1)

            def load_w(e):
                i = e % 2
                kb.dma("pool", wg[i][:], A["w_gate"][e].rearrange("(c p) n -> p c n", p=128), writes=[f"wg{i}"], sem=f"wg{i}")
                kb.dma("pool", wu[i][:], A["w_up"][e].rearrange("(c p) n -> p c n", p=128), writes=[f"wu{i}"], sem=f"wu{i}")

            load_w(0)
            kb.dma("pool", wd[:], A["w_down"][0].rearrange("(c p) n -> p c n", p=128), writes=["wd"], sem="wd")
            load_w(1)
            def scat(e, t):
                kb.dma_ins("pool", lambda t=t, e=e: nc.gpsimd.indirect_dma_start(
                    out=xin_sh, out_offset=bass.IndirectOffsetOnAxis(ap=posi[:, t, e:e + 1], axis=0),
                    in_=h3rows[:, t, :], in_offset=None, bounds_check=reg_cap, oob_is_err=False),
                    reads=["h3rows", "posi"], writes=[f"xin{e}_{t}"], sem=f"sc{e % 4}")

            first = [0, 1, 2, 3, 8, 9, 10, 11]
            later = [(e, t) for e in (4, 5, 6, 7, 12, 13, 14, 15) for t in range(HT)]
            for e in first:
                for t in range(HT):
                    scat(e, t)
            pair_barrier(2)
            def gath(e):
                xb = e % 2
                for s_ in range(4):
                    kb.dma_ins("pool", lambda s_=s_, e=e, xb=xb: nc.gpsimd.indirect_dma_start(
                        out=xsb[xb][:, s_, :], out_offset=None, in_=xin_sh,
                        in_offset=bass.IndirectOffsetOnAxis(ap=xidx[:, e * 4 + s_:e * 4 + s_ + 1], axis=0)),
                        reads=["xidx"], writes=[f"xs{xb}_{s_}"], sem=f"xs{xb}_{s_}")

            gath(0)
            for e in range(nexp):
                i = e % 2
                xs = xsb[e % 2]
                XSK = [f"xs{e % 2}_{q}" for q in range(4)]
                if e == 4:
                    pair_barrier(4)
                    gath(4)
                if e > 0:
                    kb.dma("pool", wd[:], A["w_down"][e].rearrange("(c p) n -> p c n", p=128), writes=["wd"], sem="wd")
                if 1 <= e and e + 1 < nexp:
                    load_w(e + 1)
                if e + 1 < nexp and e + 1 != 4:
                    gath(e + 1)
                if e < 4:
                    for (e2, t2) in later[e * 32:(e + 1) * 32]:
                        scat(e2, t2)
                affr = xs[:, :, 1024:1056].bitcast(F32)
                kb.op("dve", lambda affr=affr, e=e: nc.vector.tensor_tensor(gt[:, 0:4], affr[:, :, 8 + e], affr[:, :, e], ALU.subtract),
                      reads=XSK, writes=["gt"])
                kb.op("dve", lambda affr=affr, e=e: nc.vector.scalar_tensor_tensor(
                    gt[:, 4:8], gt[:, 0:4], jf[:, 0:1], affr[:, :, e], ALU.mult, ALU.add), reads=["gt", "jf"] + XSK, writes=["gt"])
                kb.op("dve", lambda xs=xs: nc.vector.tensor_copy(idf[:], xs[:, :, 1056:1058].bitcast(I32)[:, :, 0]), reads=XSK, writes=["idf"])
                kb.op("dve", lambda: nc.vector.tensor_scalar(idf[:], idf[:], joff[:, 0:1], None, ALU.add), reads=["idf", "joff"], writes=["idf"])
                kb.op("dve", lambda: nc.vector.tensor_copy(idt[:], idf[:]), reads=["idf"], writes=["idt"])
                for s_ in range(4):
                    for c in range(8):
                        kb.op("pe", lambda s_=s_, c=c, xs=xs: nc.tensor.transpose(pT[:, c, :], xs[:, s_, c * 128:(c + 1) * 128], ident_bf[:]),
                              reads=[XSK[s_], "ident_bf"], writes=["ps:pTx"], signal=(c == 7))
                    if s_ % 2 == 0:
                        kb.op("act", lambda s_=s_: nc.scalar.copy(xinT[:, :, s_ * 128:(s_ + 1) * 128], pT[:]), reads=["ps:pTx"], writes=["xinT"])
                    else:
                        kb.op("dve", lambda s_=s_: nc.vector.tensor_copy(xinT[:, :, s_ * 128:(s_ + 1) * 128], pT[:]), reads=["ps:pTx"], writes=["xinT"])
                for fc in range(8):
                    b = fc % 2
                    fs = slice(fc * 128, (fc + 1) * 128)
                    for c in range(8):
                        kb.op("pe", lambda b=b, c=c, fs=fs, i=i: nc.tensor.matmul(
                            pA[b][:], wg[i][:, c, fs], xinT[:, c, :], start=(c == 0), stop=(c == 7)),
                            reads=[f"wg{i}", "xinT"], writes=[f"ps:pGa{b}"], signal=(c == 7))
                    for c in range(8):
                        kb.op("pe", lambda b=b, c=c, fs=fs, i=i: nc.tensor.matmul(
                            pB_[b][:], wu[i][:, c, fs], xinT[:, c, :], start=(c == 0), stop=(c == 7)),
                            reads=[f"wu{i}", "xinT"], writes=[f"ps:pGb{b}"], signal=(c == 7))
                    kb.op("act", lambda b=b: nc.scalar.activation(sa[:], pA[b][:], AF.Silu), reads=[f"ps:pGa{b}"], writes=["sa"])
                    kb.op("dve", lambda b=b, fc=fc: nc.vector.tensor_tensor(hTe[:, fc, :], pB_[b][:], sa[:], ALU.mult),
                          reads=[f"ps:pGb{b}", "sa"], writes=["hTe"])
                for s_ in range(4):
                    ss_ = slice(s_ * 128, (s_ + 1) * 128)
                    gate_ap = gt[:, 4 + s_:5 + s_]
                    for half in range(2):
                        hs = slice(half * 512, (half + 1) * 512)
                        for fc in range(8):
                            kb.op("pe", lambda half=half, fc=fc, ss_=ss_, hs=hs: nc.tensor.matmul(
                                pY[half][:], hTe[:, fc, ss_], wd[:, fc, hs], start=(fc == 0), stop=(fc == 7)),
                                reads=["hTe", "wd"], writes=[f"ps:pY{half}"], signal=(fc == 7))
                        if half == 0:
                            kb.op("act", lambda hs=hs, gate_ap=gate_ap: nc.scalar.activation(
                                ysb[:, hs], pY[0][:], AF.Copy, scale=gate_ap), reads=["ps:pY0", "gt"], writes=["ysb"])
                        else:
                            kb.op("dve", lambda hs=hs, gate_ap=gate_ap: nc.vector.tensor_scalar(
                                ysb[:, hs], pY[1][:], gate_ap, None, ALU.mult), reads=["ps:pY1", "gt"], writes=["ysb"])
                    ids_ap = idt[:, s_:s_ + 1]
                    kb.dma_ins("pool", lambda ids_ap=ids_ap: nc.gpsimd.indirect_dma_start(
                        out=accp_sh, out_offset=bass.IndirectOffsetOnAxis(ap=ids_ap, axis=0),
                        in_=ysb[:], in_offset=None, bounds_check=reg_s, oob_is_err=True, compute_op=ALU.add),
                        reads=["ysb", "idt"] + [f"accz{q}" for q in range(HT)] + [f"accx{q}" for q in range(HT)],
                        writes=["accp"], sem="sa")
            pair_barrier(3)
        if upto <= 8:
            if dbg:
                dump("accp", accp_sh, [2 * S, D], F32, ["accp"])
            return finish()

        with ExitStack() as sN:
            gfin = sb(sN, "gfin", [128, D], F32)
            f0idx = sb(sN, "f0idx", [128, HT], I32)
            f1idx = sb(sN, "f1idx", [128, HT], I32)
            xf = [sb(sN, f"xf{i}", [128, D], F32) for i in range(4)]
            xg = [sb(sN, f"xg{i}", [128, D], F32) for i in range(4)]
            of = [sb(sN, f"of{i}", [128, D], F32) for i in range(4)]
            sqf = sb(sN, "sqf", [128, D], BF16)
            kb.dma("sp", gfin[:], A["g_final"], writes=["gfin"], sem="c6")
            kb.dma("sp", f0idx[:], A["f0idx"], writes=["f0idx"], sem="c6a")
            kb.dma("sp", f1idx[:], A["f1idx"], writes=["f1idx"], sem="c6b")
            for t in range(HT):
                i = t % 4
                kb.dma_ins("pool", lambda t=t, i=i: nc.gpsimd.indirect_dma_start(
                    out=xf[i][:], out_offset=None, in_=accp_sh,
                    in_offset=bass.IndirectOffsetOnAxis(ap=f0idx[:, t:t + 1], axis=0)),
                    reads=["f0idx", "accp"], writes=[f"xf{i}"], sem=f"xf{i}")
                kb.dma_ins("pool", lambda t=t, i=i: nc.gpsimd.indirect_dma_start(
                    out=xg[i][:], out_offset=None, in_=accp_sh,
                    in_offset=bass.IndirectOffsetOnAxis(ap=f1idx[:, t:t + 1], axis=0)),
                    reads=["f1idx", "accp"], writes=[f"xg{i}"], sem=f"xg{i}")
                kb.op("dve", lambda i=i: nc.vector.tensor_tensor(xf[i][:], xf[i][:], xg[i][:], ALU.add),
                      reads=[f"xf{i}", f"xg{i}"], writes=[f"xf{i}"])
                kb.op("act", lambda i=i, t=t: nc.scalar.activation(sqf[:], xf[i][:], AF.Square, accum_out=ssall[:, 5, t:t + 1]),
                      reads=[f"xf{i}"], writes=["sqf", f"ss5_{t}"])
                rstd_of(5, t, D)
                kb.op("dve", lambda i=i, t=t: nc.vector.scalar_tensor_tensor(
                    of[i][:], xf[i][:], rsall[:, 5, t:t + 1], gfin[:], ALU.mult, ALU.mult),
                    reads=[f"xf{i}", f"rs5_{t}", "gfin"], writes=[f"of{i}"])
                kb.dma("sp", out_d[t * 128:(t + 1) * 128, :], of[i][:], reads=[f"of{i}"], writes=["out"], sem=f"of{i}")
            kb.barrier()
        return finish()


_CACHE = {}


def kernel(**inputs):
    inp = {k: np.asarray(v) for k, v in inputs.items()}
    if "nc" not in _CACHE:
        _CACHE["nc"] = build()[0]
    nc = _CACHE["nc"]
    consts = host_consts()
    in_maps = []
    for core in range(8):
        m = host_layout(inp, core // 2, core % 2)
        m.update(consts)
        in_maps.append(m)
    res = run_bass_kernel_spmd(nc, in_maps, core_ids=list(range(8)))
    out = np.empty((4, S, D), np.float32)
    for core in range(8):
        out[core // 2, (core % 2) * 2048:(core % 2 + 1) * 2048] = np.asarray(res.results[core]["out"])
    return out
```
